# Optimizing a Trainium2 kernel written in Bass

```python
import math
import jax, jax.numpy as jnp
from jax import lax
import numpy as np

D_MODEL = 1024
BATCH = 8
SEQ = 2048
DEPTH = 4

D_MIX = D_MODEL
A_DIM = D_MIX // 4
A_GROUPS = 4
A_CHUNK = 128
B_HEAD_DIM = 128
B_DIM = D_MIX // 2
B_HEADS = B_DIM // B_HEAD_DIM
B_CONV = 4
B_CHUNK = 64
C_HEAD_DIM = 64
C_DIM = D_MIX - A_DIM - B_DIM
C_HEADS = C_DIM // C_HEAD_DIM
C_CONFIGS = ((128, 1), (512, 4), (2048, 16))
C_QBLOCK = 128
ROPE_THETA = 10000.0
IN_SIZES = (A_DIM, A_DIM, 3 * B_DIM, B_DIM, B_HEADS, B_HEADS, C_DIM, C_DIM, C_DIM)
D_IN = sum(IN_SIZES)
D_FF = ((8 * D_MODEL // 3 + 127) // 128) * 128
N_EXPERTS = 8
TOP_K = 2
N_DENSE = (DEPTH + 1) // 2
N_MOE = DEPTH // 2
DN_ALPHA = (2.0 * DEPTH) ** 0.25
DN_BETA = (8.0 * DEPTH) ** -0.25

kernel_name = "hybrid_gmlp_deltanet_dilated_moe_trunk"


def layer_norm(x, g, b, eps=1e-5):
    xf = x.astype(jnp.float32)
    mu = jnp.mean(xf, axis=-1, keepdims=True)
    var = jnp.mean(jnp.square(xf - mu), axis=-1, keepdims=True)
    return ((xf - mu) * lax.rsqrt(var + eps) * g + b).astype(x.dtype)


def rope(x):
    S, dh = x.shape[1], x.shape[-1]
    inv_freq = ROPE_THETA ** (-jnp.arange(0, dh, 2, dtype=jnp.float32) / dh)
    ang = jnp.arange(S, dtype=jnp.float32)[:, None] * inv_freq[None, :]
    cos = jnp.cos(ang)[None, :, None, :]
    sin = jnp.sin(ang)[None, :, None, :]
    x1 = x[..., : dh // 2].astype(jnp.float32)
    x2 = x[..., dh // 2:].astype(jnp.float32)
    return jnp.concatenate([x1 * cos - x2 * sin, x2 * cos + x1 * sin], axis=-1).astype(x.dtype)


def gmlp_spatial_gate(u, v, ln_g, ln_b, w_s, b_s):
    Bsz, S, _ = u.shape
    v = layer_norm(v, ln_g, ln_b)
    causal = jnp.tril(jnp.ones((A_CHUNK, A_CHUNK), dtype=bool))
    w = jnp.where(causal[None], w_s, jnp.zeros_like(w_s)).astype(v.dtype)
    vc = v.reshape(Bsz, S // A_CHUNK, A_CHUNK, A_GROUPS, A_DIM // A_GROUPS)
    mixed = jnp.einsum('gts,bnsgc->bntgc', w, vc) + b_s.T.astype(v.dtype)[None, None, :, :, None]
    return u * mixed.reshape(Bsz, S, A_DIM)


def causal_dwconv(x, w):
    K = w.shape[0]
    return lax.conv_general_dilated(
        x, w[:, None, :].astype(x.dtype), window_strides=(1,), padding=[(K - 1, 0)],
        dimension_numbers=('NWC', 'WIO', 'NWC'), feature_group_count=x.shape[-1])


def gated_deltanet(q, k, v, z, beta_logit, a_logit, a_log, dt_bias, norm_g):
    f32 = jnp.float32
    Bsz, S, _ = q.shape
    H, dk, C = B_HEADS, B_HEAD_DIM, B_CHUNK
    N = S // C
    q = q.reshape(Bsz, S, H, dk).astype(f32)
    k = k.reshape(Bsz, S, H, dk).astype(f32)
    v = v.reshape(Bsz, S, H, dk).astype(f32)
    q = q * lax.rsqrt(jnp.sum(q * q, -1, keepdims=True) + 1e-6) * (dk ** -0.5)
    k = k * lax.rsqrt(jnp.sum(k * k, -1, keepdims=True) + 1e-6)
    beta = jax.nn.sigmoid(beta_logit.astype(f32))
    g = -jnp.exp(a_log.astype(f32)) * jax.nn.softplus(a_logit.astype(f32) + dt_bias.astype(f32))

    def chunk(t):
        return t.reshape(Bsz, N, C, H, -1).transpose(0, 3, 1, 2, 4)

    qc, kc, vc = chunk(q), chunk(k), chunk(v)
    bc = chunk(beta[..., None])
    gc = jnp.cumsum(chunk(g[..., None])[..., 0], axis=-1)
    lower = jnp.tril(jnp.ones((C, C), dtype=bool))
    strict = jnp.tril(jnp.ones((C, C), dtype=bool), -1)
    decay = jnp.exp(jnp.where(lower, gc[..., :, None] - gc[..., None, :], -jnp.inf))
    k_beta = kc * bc
    m = jnp.where(strict, jnp.einsum('bhnid,bhnjd->bhnij', k_beta, kc) * decay, 0.0)
    eye = jnp.eye(C, dtype=f32)
    rhs = jnp.concatenate([vc * bc, k_beta * jnp.exp(gc)[..., None]], axis=-1)
    sol = lax.linalg.triangular_solve(eye + m, rhs, left_side=True, lower=True, unit_diagonal=True)
    u_c, w_c = sol[..., :dk], sol[..., dk:]
    attn_local = jnp.einsum('bhnid,bhnjd->bhnij', qc, kc) * decay
    q_dec = qc * jnp.exp(gc)[..., None]
    k_dec = kc * jnp.exp(gc[..., -1:] - gc)[..., None]
    g_last = jnp.exp(gc[..., -1])

    def step(state, xs):
        u_i, w_i, a_i, qd_i, kd_i, gl_i = xs
        v_new = u_i - jnp.einsum('bhcd,bhde->bhce', w_i, state)
        o = jnp.einsum('bhcd,bhde->bhce', qd_i, state) + jnp.einsum('bhij,bhje->bhie', a_i, v_new)
        state = state * gl_i[..., None, None] + jnp.einsum('bhcd,bhce->bhde', kd_i, v_new)
        return state, o

    xs = tuple(jnp.moveaxis(t, 2, 0) for t in (u_c, w_c, attn_local, q_dec, k_dec, g_last))
    _, o = lax.scan(step, jnp.zeros((Bsz, H, dk, dk), f32), xs)
    o = o.transpose(1, 0, 3, 2, 4).reshape(Bsz, S, H, dk)
    o = o * lax.rsqrt(jnp.mean(o * o, -1, keepdims=True) + 1e-6) * norm_g.astype(f32)
    o = o * jax.nn.silu(z.reshape(Bsz, S, H, dk).astype(f32))
    return o.reshape(Bsz, S, B_DIM).astype(z.dtype)


def dilated_attention(q, k, v):
    f32 = jnp.float32
    Bsz, S, H, dh = q.shape
    nblk = S // C_QBLOCK
    qb = (q * (dh ** -0.5)).reshape(Bsz, nblk, C_QBLOCK, H, dh).transpose(1, 0, 2, 3, 4)

    def block(args):
        blk, q_blk = args
        t = blk * C_QBLOCK + jnp.arange(C_QBLOCK)
        outs, lses = [], []
        for window, dil in C_CONFIGS:
            j = jnp.arange(window // dil + 1)
            idx = t[:, None] - dil * j[None, :]
            valid = idx >= 0
            idx = jnp.maximum(idx, 0)
            kg = jnp.take(k, idx, axis=1)
            vg = jnp.take(v, idx, axis=1)
            s = jnp.einsum('bqhd,bqjhd->bhqj', q_blk, kg).astype(f32)
            s = jnp.where(valid[None, None], s, -jnp.inf)
            mx = jnp.max(s, axis=-1, keepdims=True)
            e = jnp.exp(s - mx)
            den = jnp.sum(e, axis=-1)
            outs.append(jnp.einsum('bhqj,bqjhd->bqhd', e / den[..., None], vg.astype(f32)))
            lses.append(mx[..., 0] + jnp.log(den))
        wts = jax.nn.softmax(jnp.stack(lses, axis=0), axis=0)
        out = jnp.einsum('cbhq,cbqhd->bqhd', wts, jnp.stack(outs, axis=0))
        return out.astype(q_blk.dtype)

    out = lax.map(block, (jnp.arange(nblk), qb))
    return out.transpose(1, 0, 2, 3, 4).reshape(Bsz, S, H * dh)


def token_mixer(x, w_in, conv_w, a_ln_g, a_ln_b, a_ws, a_bs, b_a_log, b_dt_bias, b_norm_g, w_out):
    Bsz, S, _ = x.shape
    h = x @ w_in
    offs = [int(o) for o in np.cumsum(IN_SIZES)[:-1]]
    a_u, a_v, b_qkv, b_z, b_beta, b_a, c_q, c_k, c_v = jnp.split(h, offs, axis=-1)
    y_a = gmlp_spatial_gate(jax.nn.gelu(a_u), jax.nn.gelu(a_v), a_ln_g, a_ln_b, a_ws, a_bs)
    b_qkv = jax.nn.silu(causal_dwconv(b_qkv, conv_w))
    b_q, b_k, b_v = jnp.split(b_qkv, [B_DIM, 2 * B_DIM], axis=-1)
    y_b = gated_deltanet(b_q, b_k, b_v, b_z, b_beta, b_a, b_a_log, b_dt_bias, b_norm_g)
    cq = rope(c_q.reshape(Bsz, S, C_HEADS, C_HEAD_DIM))
    ck = rope(c_k.reshape(Bsz, S, C_HEADS, C_HEAD_DIM))
    cv = c_v.reshape(Bsz, S, C_HEADS, C_HEAD_DIM)
    y_c = dilated_attention(cq, ck, cv)
    return jnp.concatenate([y_a, y_b, y_c], axis=-1) @ w_out


def swiglu(x, w_gate, w_up, w_down):
    return (jax.nn.silu(x @ w_gate) * (x @ w_up)) @ w_down


def moe_swiglu(x, w_router, w_gate, w_up, w_down):
    Bsz, S, D = x.shape
    xt = x.reshape(Bsz * S, D)
    logits = (xt @ w_router).astype(jnp.float32)
    top_vals, top_idx = lax.top_k(logits, TOP_K)
    probs = jax.nn.softmax(top_vals, axis=-1)
    combine = jnp.sum(jax.nn.one_hot(top_idx, N_EXPERTS, dtype=jnp.float32) * probs[..., None], axis=1)

    def expert(acc, params):
        wg, wu, wd, c = params
        return acc + c[:, None].astype(xt.dtype) * swiglu(xt, wg, wu, wd), None

    acc, _ = lax.scan(expert, jnp.zeros_like(xt), (w_gate, w_up, w_down, combine.T))
    return acc.reshape(Bsz, S, D)


def setup_inputs(seed: int = 0) -> dict:
    key = jax.random.key(seed)
    ks = jax.random.split(key, 24)
    nrm = jax.random.normal
    x = nrm(ks[0], (BATCH, SEQ, D_MODEL), jnp.float32)
    w_in = nrm(ks[1], (DEPTH, D_MODEL, D_IN), jnp.float32) * D_MODEL ** -0.5
    conv_w = nrm(ks[2], (DEPTH, B_CONV, 3 * B_DIM), jnp.float32) * B_CONV ** -0.5
    a_ln_g = 1.0 + 0.02 * nrm(ks[3], (DEPTH, A_DIM), jnp.float32)
    a_ln_b = 0.02 * nrm(ks[4], (DEPTH, A_DIM), jnp.float32)
    a_ws = nrm(ks[5], (DEPTH, A_GROUPS, A_CHUNK, A_CHUNK), jnp.float32) * A_CHUNK ** -0.5
    a_bs = 1.0 + 0.02 * nrm(ks[6], (DEPTH, A_GROUPS, A_CHUNK), jnp.float32)
    b_a_log = jnp.log(jax.random.uniform(ks[7], (DEPTH, B_HEADS), jnp.float32, 1.0, 16.0))
    dt = jnp.exp(jax.random.uniform(ks[8], (DEPTH, B_HEADS), jnp.float32, math.log(1e-3), math.log(1e-1)))
    b_dt_bias = dt + jnp.log(-jnp.expm1(-dt))
    b_norm_g = 1.0 + 0.02 * nrm(ks[9], (DEPTH, B_HEAD_DIM), jnp.float32)
    w_out = nrm(ks[10], (DEPTH, D_MIX, D_MODEL), jnp.float32) * (D_MIX ** -0.5) * DN_BETA
    ln1_g = 1.0 + 0.02 * nrm(ks[11], (DEPTH, D_MODEL), jnp.float32)
    ln1_b = 0.02 * nrm(ks[12], (DEPTH, D_MODEL), jnp.float32)
    ln2_g = 1.0 + 0.02 * nrm(ks[13], (DEPTH, D_MODEL), jnp.float32)
    ln2_b = 0.02 * nrm(ks[14], (DEPTH, D_MODEL), jnp.float32)
    ffn_w_gate = nrm(ks[15], (N_DENSE, D_MODEL, D_FF), jnp.float32) * D_MODEL ** -0.5
    ffn_w_up = nrm(ks[16], (N_DENSE, D_MODEL, D_FF), jnp.float32) * D_MODEL ** -0.5
    ffn_w_down = nrm(ks[17], (N_DENSE, D_FF, D_MODEL), jnp.float32) * (D_FF ** -0.5) * DN_BETA
    moe_router = nrm(ks[18], (N_MOE, D_MODEL, N_EXPERTS), jnp.float32) * D_MODEL ** -0.5
    moe_w_gate = nrm(ks[19], (N_MOE, N_EXPERTS, D_MODEL, D_FF), jnp.float32) * D_MODEL ** -0.5
    moe_w_up = nrm(ks[20], (N_MOE, N_EXPERTS, D_MODEL, D_FF), jnp.float32) * D_MODEL ** -0.5
    moe_w_down = nrm(ks[21], (N_MOE, N_EXPERTS, D_FF, D_MODEL), jnp.float32) * (D_FF ** -0.5) * DN_BETA
    return {"x": x, "w_in": w_in, "conv_w": conv_w, "a_ln_g": a_ln_g, "a_ln_b": a_ln_b,
            "a_ws": a_ws, "a_bs": a_bs, "b_a_log": b_a_log, "b_dt_bias": b_dt_bias,
            "b_norm_g": b_norm_g, "w_out": w_out, "ln1_g": ln1_g, "ln1_b": ln1_b,
            "ln2_g": ln2_g, "ln2_b": ln2_b, "ffn_w_gate": ffn_w_gate, "ffn_w_up": ffn_w_up,
            "ffn_w_down": ffn_w_down, "moe_router": moe_router, "moe_w_gate": moe_w_gate,
            "moe_w_up": moe_w_up, "moe_w_down": moe_w_down}


def reference(x, w_in, conv_w, a_ln_g, a_ln_b, a_ws, a_bs, b_a_log, b_dt_bias, b_norm_g, w_out,
              ln1_g, ln1_b, ln2_g, ln2_b, ffn_w_gate, ffn_w_up, ffn_w_down,
              moe_router, moe_w_gate, moe_w_up, moe_w_down):
    for layer in range(DEPTH):
        mix = token_mixer(x, w_in[layer], conv_w[layer], a_ln_g[layer], a_ln_b[layer], a_ws[layer],
                          a_bs[layer], b_a_log[layer], b_dt_bias[layer], b_norm_g[layer], w_out[layer])
        x = layer_norm(DN_ALPHA * x + mix, ln1_g[layer], ln1_b[layer])
        i = layer // 2
        if layer % 2 == 0:
            f = swiglu(x, ffn_w_gate[i], ffn_w_up[i], ffn_w_down[i])
        else:
            f = moe_swiglu(x, moe_router[i], moe_w_gate[i], moe_w_up[i], moe_w_down[i])
        x = layer_norm(DN_ALPHA * x + f, ln2_g[layer], ln2_b[layer])
    return x
```

```python
import contextlib
import math
import numpy as np
import concourse.bass as bass
import concourse.mybir as mybir
from concourse.bass_utils import run_bass_kernel_spmd

F32 = mybir.dt.float32
BF16 = mybir.dt.bfloat16
AF = mybir.ActivationFunctionType
ALU = mybir.AluOpType
AX = mybir.AxisListType

D = 1024
SEQ = 2048
NT = 16
NG = 4
DEPTH = 4
D_IN = 3336
D_FF = 2816
NFC = 22
NE = 8
ALPHA = (2.0 * DEPTH) ** 0.25
DT_B = BF16


class Tok:
    __slots__ = ("name", "w", "r", "excl")

    def __init__(self, name="", excl=False):
        self.name = name
        self.w = None
        self.r = []
        self.excl = excl


class DSem:
    def __init__(self, name, handle):
        self.name = name
        self.handle = handle
        self.count = 0


class Sched:
    ENG = ("tensor", "vector", "scalar", "gpsimd", "sync")

    def __init__(self, nc, stack):
        self.nc = nc
        self.stack = stack
        self.gen = 0
        self.handles = {}
        self.retired = set()
        self.dsems = []
        self.ninst = {e: 0 for e in self.ENG}
        self._new_sems()

    def _new_sems(self):
        self.key = {}
        self.cnt = {}
        for e in self.ENG:
            k = f"{e}#{self.gen}"
            self.key[e] = k
            self.handles[k] = self.stack.enter_context(self.nc.semaphore(f"s_{e}_{self.gen}"))
            self.cnt[e] = 0
        self.waited = {e: {} for e in self.ENG}

    def dsem(self, name):
        h = self.stack.enter_context(self.nc.semaphore("d_" + name))
        ds = DSem("d_" + name, h)
        self.handles[ds.name] = h
        self.dsems.append(ds)
        return ds

    def _waits(self, e, reads, writes):
        need = {}
        mykey = self.key[e]

        def req(ev, same_ok):
            key, val = ev
            if key in self.retired:
                return
            if key == mykey and same_ok and e == "tensor":
                return
            if need.get(key, 0) < val:
                need[key] = val

        for t in reads:
            if t.w is not None:
                req(t.w, False)
            if t.excl:
                for ev in t.r:
                    if ev[0] != mykey:
                        req(ev, False)
        for t in writes:
            if t.w is not None:
                req(t.w, True)
            for ev in t.r:
                req(ev, True)
        out = []
        w = self.waited[e]
        for key, val in need.items():
            if w.get(key, 0) < val:
                w[key] = val
                out.append((self.handles[key], val))
        return out

    def _emit(self, e, waits, fn, sem, inc):
        eng = getattr(self.nc, e)
        for h, val in waits:
            eng.wait_ge(h, val)
            self.ninst[e] += 1
        if fn is not None:
            fn(eng).then_inc(sem, inc)
            self.ninst[e] += 1

    def op(self, e, fn, reads=(), writes=()):
        waits = self._waits(e, reads, writes)
        self.cnt[e] += 1
        ev = (self.key[e], self.cnt[e])
        self._emit(e, waits, fn, self.handles[self.key[e]], 1)
        for t in reads:
            t.r.append(ev)
            if len(t.r) > 32:
                mx = {}
                for k_, v_ in t.r:
                    if k_ not in self.retired and mx.get(k_, 0) < v_:
                        mx[k_] = v_
                t.r = list(mx.items())
        for t in writes:
            t.w = ev
            t.r = []
        return ev

    def dma(self, q, out, in_, ds, reads=(), writes=(), **kw):
        waits = self._waits(q, reads, writes)
        ds.count += 16
        ev = (ds.name, ds.count)
        self._emit(q, waits, lambda eng: eng.dma_start(out=out, in_=in_, **kw), ds.handle, 16)
        for t in reads:
            t.r.append(ev)
        for t in writes:
            t.w = ev
            t.r = []
        return ev

    def wait_events(self, e, events):
        tok = Tok("w")
        for ev in events:
            tok.w = ev
            waits = self._waits(e, [tok], [])
            self._emit(e, waits, None, None, 0)

    def barrier(self, rotate=False):
        evs = [(self.key[e], self.cnt[e]) for e in self.ENG if self.cnt[e] > 0]
        evs += [(ds.name, ds.count) for ds in self.dsems if ds.count > 0]
        for e in self.ENG:
            self.wait_events(e, evs)
        if rotate:
            for e in self.ENG:
                self.retired.add(self.key[e])
            self.gen += 1
            self._new_sems()
            for e in self.ENG:
                for ds in self.dsems:
                    self.waited[e][ds.name] = ds.count


def run_il(gens, width):
    it = iter(gens)
    active = []
    while True:
        while len(active) < width:
            try:
                active.append(next(it))
            except StopIteration:
                break
        if not active:
            break
        nxt = []
        for gn in active:
            try:
                next(gn)
                nxt.append(gn)
            except StopIteration:
                pass
        active = nxt


class RB:
    def __init__(self, alloc, name, n, shape, dt):
        self.t = [alloc(f"{name}{i}", shape, dt) for i in range(n)]
        self.k = [Tok(f"{name}{i}") for i in range(n)]
        self.i = 0

    def next(self):
        i = self.i
        self.i = (i + 1) % len(self.t)
        return self.t[i], self.k[i]


def host_consts():
    c = {}
    i = np.arange(128)
    c["ident"] = np.eye(128, dtype=np.float32)
    c["trilT"] = (i[:, None] <= i[None, :]).astype(np.float32)
    c["ones"] = np.ones((128, 128), np.float32)
    c["mneg"] = np.where(i[None, :] <= i[:, None], 0.0, 30000.0).astype(np.float32)
    c["sl"] = (i[None, :] < i[:, None]).astype(np.float32)
    inv = 10000.0 ** (-np.arange(0, 64, 2, dtype=np.float32) / 64.0)
    ang = np.arange(SEQ, dtype=np.float32)[:, None] * inv[None, :]
    c["cos"] = np.ascontiguousarray(np.cos(ang).astype(np.float32).reshape(16, 128, 32).transpose(1, 0, 2))
    c["sin"] = np.ascontiguousarray(np.sin(ang).astype(np.float32).reshape(16, 128, 32).transpose(1, 0, 2))
    u = np.arange(SEQ)[None, :] - i[:, None]
    m = ((u >= 0) & (u <= 128)).astype(np.float32)
    m += ((u >= 0) & (u <= 512) & (u % 4 == 0)).astype(np.float32)
    m += ((u >= 0) & (u <= 2048) & (u % 16 == 0)).astype(np.float32)
    c["mstrip"] = m.astype(np.float32)
    return c


CONST_SHAPES = {"ident": [128, 128], "trilT": [128, 128], "ones": [128, 128], "mneg": [128, 128],
                "sl": [128, 128], "cos": [128, 16, 32], "sin": [128, 16, 32], "mstrip": [128, SEQ]}

IN_SHAPES = {
    "x": [SEQ, D], "w_in": [DEPTH, D, D_IN], "conv_w": [DEPTH, 128, 12, 4], "a_ln_g": [DEPTH, 256],
    "a_ln_b": [DEPTH, 256], "a_ws": [DEPTH, 128, 4, 128], "a_bs": [DEPTH, 128, 4], "b_a_log": [DEPTH, 4],
    "b_dt_bias": [DEPTH, 4], "b_norm_g": [DEPTH, 128], "w_out": [DEPTH, D, D], "ln1_g": [DEPTH, D],
    "ln1_b": [DEPTH, D], "ln2_g": [DEPTH, D], "ln2_b": [DEPTH, D], "ffn_w_gate": [2, D, D_FF],
    "ffn_w_up": [2, D, D_FF], "ffn_w_down": [2, D_FF, D], "moe_router": [2, D, NE],
    "moe_w_gate": [2, NE, D, D_FF], "moe_w_up": [2, NE, D, D_FF], "moe_w_down": [2, NE, D_FF, D],
}


import os
KDBG = os.environ.get("KDBG", "")


def small_shape(k, shp, n_layers):
    if n_layers >= DEPTH or k == "x":
        return list(shp)
    shp = list(shp)
    if k.startswith("ffn_"):
        shp[0] = (n_layers + 1) // 2
        if "nof" in KDBG:
            shp = [1, 8, 8]
    elif k.startswith("moe_"):
        shp[0] = max(1, n_layers // 2)
        if n_layers < 2:
            shp = [1] + [1] * (len(shp) - 3) + shp[-2:] if len(shp) == 4 else shp
    else:
        shp[0] = n_layers
    return shp


def build(n_layers=DEPTH, steps="ACBF", dbg=False):
    nc = bass.Bass("TRN2", target_bir_lowering=False)
    I = {}
    for k, shp in list(IN_SHAPES.items()):
        I[k] = nc.dram_tensor(k, small_shape(k, shp, n_layers), F32, kind="ExternalInput").ap()
    for k, shp in list(CONST_SHAPES.items()):
        I[k] = nc.dram_tensor(k, shp, F32, kind="ExternalInput").ap()
    out = nc.dram_tensor("out", [SEQ, D], F32, kind="ExternalOutput").ap()

    with contextlib.ExitStack() as gst:
        S = Sched(nc, gst)

        def galloc(n, s, d):
            return gst.enter_context(nc.sbuf_tensor(n, s, d))

        X = galloc("X", [128, NT, D], F32)
        XT = galloc("XT", [128, 8, SEQ], BF16)
        tX = [Tok(f"X{t}") for t in range(NT)]
        tXT = [Tok(f"XT{g}") for g in range(NG)]
        ident_f = galloc("ident_f", [128, 128], F32)
        ident_b = galloc("ident_b", [128, 128], BF16)
        trilT_f = galloc("trilT_f", [128, 128], F32)
        ones_f = galloc("ones_f", [128, 128], F32)
        mneg_f = galloc("mneg_f", [128, 128], F32)
        sl_b = galloc("sl_b", [128, 128], DT_B)
        tC = Tok("consts")
        banks = [gst.enter_context(nc.psum_tensor(f"bank{i}", [128, 512], F32)) for i in range(8)]
        bank_tok = [Tok(f"bank{i}", excl=True) for i in range(8)]
        bstate = {"i": 0}

        def nb():
            i = bstate["i"]
            bstate["i"] = (i + 1) % 6
            return banks[i], bank_tok[i]

        def nb_special():
            i = 6 + bstate.get("s", 0)
            bstate["s"] = bstate.get("s", 0) ^ 1
            return banks[i], bank_tok[i]

        def mm(o, lhsT, rhs, r, w, start=True, stop=True, sgc=False):
            S.op("tensor", lambda e: e.matmul(o, lhsT=lhsT, rhs=rhs, start=start, stop=stop, skip_group_check=sgc), r, w)

        def tr(o, i_, idn, r, w):
            S.op("tensor", lambda e: e.transpose(out=o, in_=i_, identity=idn), r, w)

        def act(o, i_, func, r, w, **kw):
            S.op("scalar", lambda e: e.activation(out=o, in_=i_, func=func, **kw), r, w)

        def ts(o, i0, s1, s2, op0, op1, r, w, eng="vector"):
            if op1 is None:
                S.op(eng, lambda e: e.tensor_scalar(out=o, in0=i0, scalar1=s1, scalar2=None, op0=op0), r, w)
            else:
                S.op(eng, lambda e: e.tensor_scalar(out=o, in0=i0, scalar1=s1, scalar2=s2, op0=op0, op1=op1), r, w)

        def tt(o, i0, i1, op, r, w, eng="vector"):
            S.op(eng, lambda e: e.tensor_tensor(out=o, in0=i0, in1=i1, op=op), r, w)

        def stt(o, i0, sc, i1, op0, op1, r, w):
            S.op("vector", lambda e: e.scalar_tensor_tensor(out=o, in0=i0, scalar=sc, in1=i1, op0=op0, op1=op1), r, w)

        def cp(o, i_, r, w, eng="vector"):
            if eng == "scalar":
                S.op("scalar", lambda e: e.copy(out=o, in_=i_), r, w)
            else:
                S.op(eng, lambda e: e.tensor_copy(out=o, in_=i_), r, w)

        dconst = S.dsem("const")
        dx = S.dsem("x")
        dw = [S.dsem(f"w{i}") for i in range(6)]
        dout = S.dsem("out")

        with contextlib.ExitStack() as st0:
            sl_f = st0.enter_context(nc.sbuf_tensor("sl_f", [128, 128], F32))
            tmpk = Tok()
            S.dma("sync", ident_f[:], I["ident"], dconst, writes=[tC])
            S.dma("sync", trilT_f[:], I["trilT"], dconst, writes=[tC])
            S.dma("sync", ones_f[:], I["ones"], dconst, writes=[tC])
            S.dma("sync", mneg_f[:], I["mneg"], dconst, writes=[tC])
            S.dma("sync", sl_f[:], I["sl"], dconst, writes=[tmpk])
            tC.w = (dconst.name, dconst.count)
            tmpk.w = (dconst.name, dconst.count)
            cp(ident_b[:], ident_f[:], [tC], [tC])
            cp(sl_b[:], sl_f[:], [tmpk], [tC])
            xv = I["x"].rearrange("(t p) d -> p t d", p=128)
            for q in range(4):
                S.dma("sync" if q % 2 == 0 else "scalar", X[:, 4 * q:4 * q + 4, :], xv[:, 4 * q:4 * q + 4, :], dx,
                      writes=tX[4 * q:4 * q + 4])
            for t in range(NT):
                tX[t].w = (dx.name, dx.count)
            S.barrier()

        def make_xt(g, router=None, scale=None):
            for c in range(8):
                bk, bt = nb()
                for q in range(4):
                    t = 4 * g + q
                    tr(bk[:, q * 128:(q + 1) * 128], X[:, t, c * 128:(c + 1) * 128], ident_f[:], [tX[t], tC], [bt])
                dst = XT[:, c, g * 512:(g + 1) * 512]
                if scale is None:
                    cp(dst, bk[:], [bt], [tXT[g]], eng=("scalar" if c % 2 == 0 else "vector"))
                elif c % 2 == 0:
                    act(dst, bk[:], AF.Copy, [bt], [tXT[g]], scale=scale)
                else:
                    ts(dst, bk[:], scale, None, ALU.mult, None, [bt], [tXT[g]])
                if router is not None:
                    router(g, c, bk, bt)

        def ln_gen(t, gam, bet, tpar, sm, eps=1e-5, after=None):
            st6, mv, sd, rs, ksm = sm
            for hf in range(2):
                S.op("vector", lambda e: e.bn_stats(out=st6[:, t, hf, :], in_=X[:, t, hf * 512:(hf + 1) * 512]),
                     [tX[t]], [ksm[t]])
            S.op("vector", lambda e: e.bn_aggr(out=mv[:, t, :], in_=st6[:, t, :, :]), [ksm[t]], [ksm[t]])
            yield
            act(sd[:, t:t + 1], mv[:, t, 1:2], AF.Sqrt, [ksm[t]], [ksm[t]], bias=eps, scale=1.0)
            yield
            S.op("vector", lambda e: e.reciprocal(out=rs[:, t:t + 1], in_=sd[:, t:t + 1]), [ksm[t]], [ksm[t]])
            ts(sd[:, t:t + 1], mv[:, t, 0:1], -1.0, rs[:, t:t + 1], ALU.mult, ALU.mult, [ksm[t]], [ksm[t]])
            yield
            act(X[:, t, :], X[:, t, :], AF.Identity, [tX[t], ksm[t]], [tX[t]], scale=rs[:, t:t + 1], bias=sd[:, t:t + 1])
            yield
            tt(X[:, t, :], X[:, t, :], gam[:], ALU.mult, [tX[t], tpar], [tX[t]])
            yield
            tt(X[:, t, :], X[:, t, :], bet[:], ALU.add, [tX[t], tpar], [tX[t]], eng="gpsimd")
            if after is not None:
                yield
                after(t)

        def accum_X(t, hf, bk, bt, first, cscal=None):
            xs = X[:, t, hf * 512:(hf + 1) * 512]
            if first:
                stt(xs, xs, ALPHA, bk[:], ALU.mult, ALU.add, [tX[t], bt], [tX[t]])
            elif cscal is not None:
                stt(xs, bk[:], cscal, xs, ALU.mult, ALU.add, [tX[t], bt], [tX[t]])
            else:
                tt(xs, bk[:], xs, ALU.add, [tX[t], bt], [tX[t]])

        def outproj(t, yT, kyT, Wo, kWo, nk, first):
            for hf in range(2):
                bk, bt = nb()
                for k in range(nk):
                    mm(bk[:], yT[:, k, :], Wo[:, k, hf * 512:(hf + 1) * 512], [kyT, kWo], [bt],
                       start=(k == 0), stop=(k == nk - 1))
                accum_X(t, hf, bk, bt, first)

        def transpose_bf(src, ksrc, nk, dst, kdst, eng="scalar"):
            bk, bt = nb()
            bkb = bk[:].bitcast(BF16)
            for k in range(nk):
                tr(bkb[:, k * 128:(k + 1) * 128], src[:, k * 128:(k + 1) * 128], ident_b[:], [ksrc, tC], [bt])
            cp(dst[:], bkb[:, 0:nk * 128].rearrange("p (k c) -> p k c", k=nk), [bt], [kdst], eng=eng)

        for g in range(NG):
            make_xt(g)

        for l in range(n_layers):
            winv = I["w_in"][l].rearrange("(c p) f -> p c f", p=128)
            woutv = I["w_out"][l].rearrange("(c p) f -> p c f", p=128)
            first_acc = [True] * NT

            if "A" in steps:
                with contextlib.ExitStack() as st:
                    def al(n, s, d):
                        return st.enter_context(nc.sbuf_tensor(f"A{l}_{n}", s, d))
                    WA = al("WA", [128, 8, 512], BF16); kWA = Tok()
                    WoA = al("WoA", [128, 2, D], BF16); kWo = Tok()
                    Wsf = al("Wsf", [128, 4, 128], F32)
                    WsT = al("WsT", [128, 4, 128], BF16); kWs = Tok()
                    bsA = al("bsA", [128, 4], F32)
                    lng = al("lng", [128, 256], F32)
                    lnb = al("lnb", [128, 256], F32); kP = Tok()
                    S.dma("gpsimd", WA[:], winv[:, :, 0:512], dw[0], writes=[kWA])
                    S.dma("gpsimd", WoA[:], woutv[:, 0:2, :], dw[1], writes=[kWo])
                    S.dma("sync", Wsf[:], I["a_ws"][l], dw[2], writes=[kWs])
                    S.dma("sync", bsA[:], I["a_bs"][l], dw[2], writes=[kP])
                    S.dma("sync", lng[:], I["a_ln_g"][l:l + 1, :].broadcast_to([128, 256]), dw[2], writes=[kP])
                    S.dma("sync", lnb[:], I["a_ln_b"][l:l + 1, :].broadcast_to([128, 256]), dw[2], writes=[kP])
                    kWs.w = (dw[2].name, dw[2].count)
                    kP.w = (dw[2].name, dw[2].count)
                    tt(WsT[:], Wsf[:], trilT_f[:].unsqueeze(1).broadcast_to([128, 4, 128]), ALU.mult, [kWs, tC], [kWs])
                    WIL = 4
                    sq = RB(al, "sq", WIL, [128, 512], F32)
                    inn = RB(al, "inn", WIL, [128, 512], F32)
                    ge = RB(al, "ge", WIL, [128, 512], F32)
                    vn = RB(al, "vn", WIL, [128, 256], BF16)
                    ya = RB(al, "ya", WIL, [128, 256], BF16)
                    yaT = RB(al, "yaT", WIL, [128, 2, 128], BF16)
                    sm = RB(al, "smA", WIL, [128, 16], F32)

                    def a_tile(t):
                        g = t // 4
                        p1, k1 = nb()
                        for c in range(8):
                            mm(p1[:], XT[:, c, t * 128:(t + 1) * 128], WA[:, c, :], [tXT[g], kWA], [k1],
                               start=(c == 0), stop=(c == 7))
                        sq_, ksq = sq.next(); in_, kin = inn.next(); ge_, kge = ge.next()
                        yield
                        act(sq_[:], p1[:], AF.Square, [k1], [ksq])
                        yield
                        ts(in_[:], sq_[:], 0.044715, 1.0, ALU.mult, ALU.add, [ksq], [kin])
                        tt(in_[:], in_[:], p1[:], ALU.mult, [kin, k1], [kin])
                        yield
                        act(sq_[:], in_[:], AF.Sigmoid, [kin], [ksq], scale=1.5957691216057308)
                        yield
                        tt(ge_[:], sq_[:], p1[:], ALU.mult, [ksq, k1], [kge])
                        s_, ks = sm.next()
                        S.op("vector", lambda e: e.bn_stats(out=s_[:, 0:6], in_=ge_[:, 256:512]), [kge], [ks])
                        S.op("vector", lambda e: e.bn_aggr(out=s_[:, 6:8], in_=s_[:, 0:6]), [ks], [ks])
                        yield
                        act(s_[:, 8:9], s_[:, 7:8], AF.Sqrt, [ks], [ks], bias=1e-5, scale=1.0)
                        yield
                        S.op("vector", lambda e: e.reciprocal(out=s_[:, 9:10], in_=s_[:, 8:9]), [ks], [ks])
                        ts(in_[:, 0:256], ge_[:, 256:512], s_[:, 6:7], s_[:, 9:10], ALU.subtract, ALU.mult,
                           [kge, ks], [kin])
                        yield
                        tt(in_[:, 0:256], in_[:, 0:256], lng[:], ALU.mult, [kin, kP], [kin], eng="gpsimd")
                        vn_, kvn = vn.next()
                        tt(vn_[:], in_[:, 0:256], lnb[:], ALU.add, [kin, kP], [kvn], eng="gpsimd")
                        yield
                        p2, k2 = nb()
                        for gi in range(4):
                            mm(p2[:, gi * 64:(gi + 1) * 64], WsT[:, gi, :], vn_[:, gi * 64:(gi + 1) * 64], [kWs, kvn], [k2])
                        yield
                        ya_, kya = ya.next()
                        for gi in range(4):
                            stt(ya_[:, gi * 64:(gi + 1) * 64], p2[:, gi * 64:(gi + 1) * 64], bsA[:, gi:gi + 1],
                                ge_[:, gi * 64:(gi + 1) * 64], ALU.add, ALU.mult, [k2, kP, kge], [kya])
                        yield
                        yT_, kyT = yaT.next()
                        transpose_bf(ya_, kya, 2, yT_, kyT)
                        yield
                        outproj(t, yT_, kyT, WoA, kWo, 2, first_acc[t])
                        first_acc[t] = False

                    run_il([a_tile(t) for t in range(NT)], WIL)
                    S.barrier()

            if "C" in steps:
                with contextlib.ExitStack() as st:
                    def al(n, s, d):
                        return st.enter_context(nc.sbuf_tensor(f"C{l}_{n}", s, d))
                    WC = al("WC", [128, 8, 768], BF16); kWC = Tok()
                    WoC = al("WoC", [128, 2, D], BF16); kWo = Tok()
                    QT = al("QT", [128, 2, SEQ], BF16); kQT = [Tok() for _ in range(NT)]
                    KT = al("KT", [128, 2, SEQ], BF16); kKT = [Tok() for _ in range(NT)]
                    Vp = al("Vp", [128, NT, 4, 65], BF16); kV = [Tok() for _ in range(NT)]
                    Mst = al("Mst", [128, SEQ], BF16); kM = Tok()
                    cosT = al("cosT", [128, NT, 32], F32)
                    sinT = al("sinT", [128, NT, 32], F32); kcs = Tok()
                    S.dma("gpsimd", WC[:], winv[:, :, 2568:3336], dw[0], writes=[kWC])
                    S.dma("gpsimd", WoC[:], woutv[:, 6:8, :], dw[1], writes=[kWo])
                    S.dma("gpsimd", Mst[:, 0:1024], I["mstrip"][:, 0:1024], dw[3], writes=[kM])
                    S.dma("gpsimd", Mst[:, 1024:2048], I["mstrip"][:, 1024:2048], dw[3], writes=[kM])
                    kM.w = (dw[3].name, dw[3].count)
                    S.dma("sync", cosT[:], I["cos"], dw[2], writes=[kcs])
                    S.dma("sync", sinT[:], I["sin"], dw[2], writes=[kcs])
                    kcs.w = (dw[2].name, dw[2].count)
                    kVall = Tok()
                    S.op("gpsimd", lambda e: e.memset(Vp[:], 1.0), [], [kVall])
                    for t in range(NT):
                        kV[t].w = kVall.w
                    qf = RB(al, "qf", 4, [128, 256], F32)
                    kf = RB(al, "kf", 4, [128, 256], F32)
                    r1 = RB(al, "r1", 8, [128, 4, 32], F32)
                    r2 = RB(al, "r2", 8, [128, 4, 32], F32)
                    r3 = RB(al, "r3", 8, [128, 4, 32], F32)
                    r4 = RB(al, "r4", 8, [128, 4, 32], F32)
                    qr = RB(al, "qr", 4, [128, 256], BF16)
                    kr = RB(al, "kr", 4, [128, 256], BF16)
                    Pb = RB(al, "Pb", 6, [128, 512], BF16)
                    YC = RB(al, "YC", 2, [128, 4, 256], BF16)
                    ycT = RB(al, "ycT", 2, [128, 2, 128], BF16)
                    rc = RB(al, "rc", 4, [128, 4], F32)

                    def rope(src, ksrc, dst, kdst, t):
                        x4 = src[:].rearrange("p (h a i) -> p h a i", h=4, a=2)
                        o4 = dst[:].rearrange("p (h a i) -> p h a i", h=4, a=2)
                        x1 = x4[:, :, 0, :]; x2 = x4[:, :, 1, :]
                        cb = cosT[:, t:t + 1, :].broadcast_to([128, 4, 32])
                        sb_ = sinT[:, t:t + 1, :].broadcast_to([128, 4, 32])
                        a_, ka = r1.next(); b_, kb = r2.next(); c_, kc = r3.next(); d_, kd = r4.next()
                        tt(a_[:], x1, cb, ALU.mult, [ksrc, kcs], [ka])
                        tt(b_[:], x2, sb_, ALU.mult, [ksrc, kcs], [kb])
                        tt(c_[:], x2, cb, ALU.mult, [ksrc, kcs], [kc], eng="gpsimd")
                        tt(d_[:], x1, sb_, ALU.mult, [ksrc, kcs], [kd], eng="gpsimd")
                        tt(o4[:, :, 0, :], a_[:], b_[:], ALU.subtract, [ka, kb], [kdst])
                        tt(o4[:, :, 1, :], c_[:], d_[:], ALU.add, [kc, kd], [kdst])

                    def c_proj(t):
                        g = t // 4
                        pq, kpq = nb()
                        for c in range(8):
                            mm(pq[:, 0:256], XT[:, c, t * 128:(t + 1) * 128], WC[:, c, 0:256], [tXT[g], kWC], [kpq],
                               start=(c == 0), stop=(c == 7))
                        pkv, kpkv = nb()
                        for c in range(8):
                            mm(pkv[:], XT[:, c, t * 128:(t + 1) * 128], WC[:, c, 256:768], [tXT[g], kWC], [kpkv],
                               start=(c == 0), stop=(c == 7))
                        qf_, kqf = qf.next(); kf_, kkf = kf.next()
                        yield
                        act(qf_[:], pq[:, 0:256], AF.Copy, [kpq], [kqf], scale=0.125)
                        act(kf_[:], pkv[:, 0:256], AF.Copy, [kpkv], [kkf])
                        cp(Vp[:, t, :, 0:64], pkv[:, 256:512].rearrange("p (h d) -> p h d", h=4), [kpkv], [kV[t]])
                        qr_, kqr = qr.next(); kr_, kkr = kr.next()
                        yield
                        rope(qf_, kqf, qr_, kqr, t)
                        yield
                        rope(kf_, kkf, kr_, kkr, t)
                        yield
                        bk, bt = nb()
                        bkb = bk[:].bitcast(BF16)
                        for k in range(2):
                            tr(bkb[:, k * 128:(k + 1) * 128], qr_[:, k * 128:(k + 1) * 128], ident_b[:], [kqr, tC], [bt])
                        for k in range(2):
                            tr(bkb[:, 256 + k * 128:256 + (k + 1) * 128], kr_[:, k * 128:(k + 1) * 128], ident_b[:],
                               [kkr, tC], [bt])
                        yield
                        cp(QT[:, :, t * 128:(t + 1) * 128], bkb[:, 0:256].rearrange("p (k c) -> p k c", k=2), [bt],
                           [kQT[t]], eng="scalar")
                        cp(KT[:, :, t * 128:(t + 1) * 128], bkb[:, 256:512].rearrange("p (k c) -> p k c", k=2), [bt],
                           [kKT[t]], eng="vector")

                    mask_flip = [0]

                    def attn_head(g, h, yc_, kyc):
                        hp = h // 2; hb = 64 * (h % 2)
                        ob, ko = nb_special()
                        ov = ob[:, 0:260].rearrange("p (i e) -> p i e", e=65)
                        nj = 4 * g + 4

                        def score(j):
                            i0 = max(j, 4 * g)
                            n = (4 * g + 4 - i0) * 128
                            sc, ksc = nb()
                            mm(sc[:, 0:n], KT[hb:hb + 64, hp, j * 128:(j + 1) * 128],
                               QT[hb:hb + 64, hp, i0 * 128:(4 * g + 4) * 128],
                               [kKT[j]] + kQT[i0:4 * g + 4], [ksc])
                            return sc, ksc, i0, n

                        firstmm = True
                        pend = score(0)
                        for j in range(nj):
                            sc, ksc, i0, n = pend
                            p_, kp = Pb.next()
                            act(p_[:, 0:n], sc[:, 0:n], AF.Exp, [ksc], [kp])
                            if j + 1 < nj:
                                pend = score(j + 1)
                            yield
                            u0 = 128 * (i0 - j)
                            mask_flip[0] = (mask_flip[0] + 1) % 3
                            tt(p_[:, 0:n], p_[:, 0:n], Mst[:, u0:u0 + n], ALU.mult, [kp, kM], [kp],
                               eng=("gpsimd" if mask_flip[0] == 0 else "vector"))
                            yield
                            for i in range(i0, 4 * g + 4):
                                mm(ov[:, i - 4 * g, :], p_[:, (i - i0) * 128:(i - i0 + 1) * 128], Vp[:, j, h, :],
                                   [kp, kV[j]], [ko], start=firstmm, stop=(j == i), sgc=True)
                                firstmm = False
                            yield
                        rc_, krc = rc.next()
                        S.op("vector", lambda e: e.reciprocal(out=rc_[:], in_=ov[:, :, 64]), [ko], [krc])
                        for i in range(4):
                            ts(yc_[:, i, h * 64:(h + 1) * 64], ov[:, i, 0:64], rc_[:, i:i + 1], None, ALU.mult, None,
                               [ko, krc], [kyc])

                    def attn_group(g):
                        yc_, kyc = YC.next()
                        if "Cnoh1" in KDBG:
                            run_il([attn_head(g, h, yc_, kyc) for h in (0, 2)], 2)
                        else:
                            run_il([attn_head(g, h, yc_, kyc) for h in range(4)], 2)
                        for i in range(4):
                            t = 4 * g + i
                            yT_, kyT = ycT.next()
                            transpose_bf(yc_[:, i, :], kyc, 2, yT_, kyT)
                            outproj(t, yT_, kyT, WoC, kWo, 2, first_acc[t])
                            first_acc[t] = False

                    for g in range(NG):
                        run_il([c_proj(4 * g + q) for q in range(4)], 2)
                        if "Cproj" not in KDBG:
                            attn_group(g)
                    S.barrier()

            if "B" in steps:
                for hp2 in range(2):
                    with contextlib.ExitStack() as st:
                        def al(n, s, d):
                            return st.enter_context(nc.sbuf_tensor(f"B{l}{hp2}_{n}", s, d))
                        idn = ident_f if DT_B == F32 else ident_b
                        Wq = al("Wq", [128, 8, 768], BF16); kWq = Tok()
                        Wz = al("Wz", [128, 8, 260], BF16); kWz = Tok()
                        WoB = al("WoB", [128, 2, D], BF16); kWo = Tok()
                        cw = al("cw", [128, 12, 4], F32)
                        ngt = al("ngt", [128, 128], F32)
                        alog = al("alog", [128, 4], F32)
                        dtb = al("dtb", [128, 4], F32)
                        nexpA = al("nexpA", [128, 4], F32); kP = Tok()
                        for pi, base in enumerate((512, 1024, 1536)):
                            S.dma("gpsimd", Wq[:, :, pi * 256:(pi + 1) * 256],
                                  winv[:, :, base + hp2 * 256: base + (hp2 + 1) * 256], dw[0], writes=[kWq])
                        kWq.w = (dw[0].name, dw[0].count)
                        S.dma("gpsimd", Wz[:, :, 0:256], winv[:, :, 2048 + hp2 * 256:2048 + (hp2 + 1) * 256], dw[1], writes=[kWz])
                        S.dma("gpsimd", Wz[:, :, 256:258], winv[:, :, 2560 + 2 * hp2:2562 + 2 * hp2], dw[1], writes=[kWz])
                        S.dma("gpsimd", Wz[:, :, 258:260], winv[:, :, 2564 + 2 * hp2:2566 + 2 * hp2], dw[1], writes=[kWz])
                        kWz.w = (dw[1].name, dw[1].count)
                        S.dma("gpsimd", WoB[:], woutv[:, 2 + 2 * hp2:4 + 2 * hp2, :], dw[3], writes=[kWo])
                        S.dma("sync", cw[:], I["conv_w"][l], dw[2], writes=[kP])
                        S.dma("sync", ngt[:], I["b_norm_g"][l:l + 1, :].broadcast_to([128, 128]), dw[2], writes=[kP])
                        S.dma("sync", alog[:], I["b_a_log"][l:l + 1, :].broadcast_to([128, 4]), dw[2], writes=[kP])
                        S.dma("sync", dtb[:], I["b_dt_bias"][l:l + 1, :].broadcast_to([128, 4]), dw[2], writes=[kP])
                        kP.w = (dw[2].name, dw[2].count)
                        act(nexpA[:], alog[:], AF.Exp, [kP], [kP])
                        ts(nexpA[:], nexpA[:], -1.0, None, ALU.mult, None, [kP], [kP])

                        NCH = 4
                        NS = 2 * NCH
                        halo = al("halo", [128, 6, 3], F32); khalo = [Tok() for _ in range(6)]
                        S.op("gpsimd", lambda e: e.memset(halo[:], 0.0), [], khalo)
                        Hb = RB(al, "Hb", 2, [128, 515], F32)
                        cacc = RB(al, "cacc", 1, [128, 512], F32)
                        QfT = RB(al, "QfT", 2, [128, 2, 512], DT_B)
                        tmpS = RB(al, "tmpS", 2, [128, 512], DT_B)
                        QKVt = al("QKVt", [128, 4, 768], DT_B); kQKV = [Tok() for _ in range(4)]
                        Sst = al("Sst", [128, 2, 128], F32); kS = [Tok(), Tok()]
                        if DT_B != F32:
                            Sbf = al("Sbf", [128, 2, 128], DT_B)
                        else:
                            Sbf = Sst
                        zng = RB(al, "zng", 2 * NCH, [128, 256], BF16)
                        zs = RB(al, "zs", 2, [128, 256], F32)
                        gsm = RB(al, "gsm", 3, [128, 160], F32)
                        sqj = RB(al, "sqj", 2, [128, 512], BF16)
                        yb = RB(al, "yb", 2, [128, 256], BF16)
                        ybT = RB(al, "ybT", 2, [128, 2, 128], BF16)

                        def mk(nm, n, dt=DT_B):
                            return RB(al, nm, n, [128, 128], dt)
                        kn_b = mk("kn", NS); knT_b = mk("knT", NS); E_b = mk("E", NS); Es_b = mk("Es", NS)
                        at_b = mk("attn", NS); kbg_b = mk("kbg", NS); dg_b = mk("dg", 8, F32)
                        A_b = mk("Ab", 4 * NS); P_b = mk("Pb", 2 * NS); TT_b = mk("TTb", 2 * NS)
                        kdec_b = mk("kdec", 2 * NS); vb_b = mk("vb", 2 * NS); atT_b = mk("attnT", 2 * NS); nwT_b = mk("nwT", 2 * NS)
                        vnew_b = mk("vnew", 4); o_b = mk("o", 4, F32); otmp_b = mk("otmp", 4, F32); junk_b = mk("junk", 4, F32)

                        def psv(bk):
                            return bk[:] if DT_B == F32 else bk[:].bitcast(BF16)

                        def b1(g):
                            qf_, kqf = QfT.next()
                            for ci in range(6):
                                part = ci // 2
                                ct = part * 4 + hp2 * 2 + (ci % 2)
                                bk, bt = nb()
                                for c in range(8):
                                    mm(bk[:], Wq[:, c, ci * 128:(ci + 1) * 128], XT[:, c, g * 512:(g + 1) * 512],
                                       [kWq, tXT[g]], [bt], start=(c == 0), stop=(c == 7))
                                H, kH = Hb.next()
                                cp(H[:, 0:3], halo[:, ci, :], [khalo[ci]], [kH], eng="gpsimd")
                                act(H[:, 3:515], bk[:], AF.Copy, [bt], [kH])
                                cp(halo[:, ci, :], H[:, 512:515], [kH], [khalo[ci]], eng="gpsimd")
                                ac, kac = cacc.next()
                                ts(ac[:], H[:, 0:512], cw[:, ct, 0:1], None, ALU.mult, None, [kH, kP], [kac])
                                for k in range(1, 4):
                                    stt(ac[:], H[:, k:k + 512], cw[:, ct, k:k + 1], ac[:], ALU.mult, ALU.add, [kH, kP, kac], [kac])
                                if ci < 2:
                                    dst = qf_[:, ci, :]; kd = kqf
                                else:
                                    d_, kd = tmpS.next(); dst = d_[:]
                                act(dst, ac[:], AF.Silu, [kac], [kd])
                                bk2, bt2 = nb()
                                bv = psv(bk2)
                                for q in range(4):
                                    tr(bv[:, q * 128:(q + 1) * 128], dst[:, q * 128:(q + 1) * 128], idn[:], [kd, tC], [bt2])
                                cp(QKVt[:, :, ci * 128:(ci + 1) * 128], bv[:, 0:512].rearrange("p (q c) -> p q c", q=4),
                                   [bt2], kQKV, eng=("vector" if ci % 2 else "scalar"))
                            return qf_, kqf

                        FB, FG, FGC, FGS, FEG, FER, FEL, FSQ, FSK, FRQ0, FRK, FRKB, FRKBG, FRKD, FNB, FRQ, FRQE, FSSO, FRST = range(19)

                        def col(f, q, hl):
                            return f * 8 + q * 2 + hl

                        def gates_group(g):
                            s_, ks = gsm.next()
                            zns = []
                            bks = []
                            for q in range(4):
                                t = 4 * g + q
                                bk, bt = nb()
                                for c in range(8):
                                    mm(bk[:, 0:260], XT[:, c, t * 128:(t + 1) * 128], Wz[:, c, :], [tXT[g], kWz], [bt],
                                       start=(c == 0), stop=(c == 7))
                                zs_, kzs = zs.next(); zn_, kzn = zng.next()
                                act(zs_[:], bk[:, 0:256], AF.Silu, [bt], [kzs])
                                cp(s_[:, col(FB, q, 0):col(FB, q, 0) + 2], bk[:, 256:258], [bt], [ks])
                                tt(s_[:, col(FG, q, 0):col(FG, q, 0) + 2], bk[:, 258:260], dtb[:, 2 * hp2:2 * hp2 + 2], ALU.add,
                                   [bt, kP], [ks])
                                tt(zn_[:].rearrange("p (h d) -> p h d", h=2), zs_[:].rearrange("p (h d) -> p h d", h=2),
                                   ngt[:].unsqueeze(1).broadcast_to([128, 2, 128]), ALU.mult, [kzs, kP], [kzn])
                                zns.append((zn_, kzn))
                            for q in range(4):
                                jk, kjk = sqj.next()
                                act(jk[:], QKVt[:, q, 0:512], AF.Square, [kQKV[q]], [kjk])
                                S.op("vector", lambda e: e.tensor_reduce(out=s_[:, col(FSQ, q, 0):col(FSQ, q, 0) + 2],
                                                                         in_=jk[:, 0:256].rearrange("p (h d) -> p h d", h=2),
                                                                         axis=AX.X, op=ALU.add), [kjk, ks], [ks])
                                S.op("vector", lambda e: e.tensor_reduce(out=s_[:, col(FSK, q, 0):col(FSK, q, 0) + 2],
                                                                         in_=jk[:, 256:512].rearrange("p (h d) -> p h d", h=2),
                                                                         axis=AX.X, op=ALU.add), [kjk, ks], [ks])
                            f8 = lambda f: s_[:, f * 8:(f + 1) * 8]
                            act(f8(FB), f8(FB), AF.Exp, [ks], [ks], scale=-1.0)
                            ts(f8(FB), f8(FB), 1.0, None, ALU.add, None, [ks], [ks])
                            S.op("vector", lambda e: e.reciprocal(out=f8(FB), in_=f8(FB)), [ks], [ks])
                            act(f8(FG), f8(FG), AF.Exp, [ks], [ks])
                            act(f8(FG), f8(FG), AF.Ln, [ks], [ks], bias=1.0, scale=1.0)
                            tt(f8(FG).rearrange("p (q h) -> p q h", h=2), f8(FG).rearrange("p (q h) -> p q h", h=2),
                               nexpA[:, 2 * hp2:2 * hp2 + 2].unsqueeze(1).broadcast_to([128, 4, 2]), ALU.mult, [ks, kP], [ks])
                            bg, kbg_ = nb()
                            mm(bg[:, 0:8], trilT_f[:], f8(FG), [tC, ks], [kbg_])
                            mm(bg[:, 8:16], ones_f[:], f8(FG), [tC, ks], [kbg_])
                            cp(s_[:, FGC * 8:FGC * 8 + 16], bg[:, 0:16], [kbg_], [ks])
                            act(f8(FEG), f8(FGC), AF.Exp, [ks], [ks])
                            tt(f8(FER), f8(FGS), f8(FGC), ALU.subtract, [ks], [ks])
                            act(f8(FER), f8(FER), AF.Exp, [ks], [ks])
                            act(f8(FEL), f8(FGS), AF.Exp, [ks], [ks])
                            act(s_[:, FRQ0 * 8:FRQ0 * 8 + 16], s_[:, FSQ * 8:FSQ * 8 + 16], AF.Ln, [ks], [ks], bias=1e-6, scale=1.0)
                            act(s_[:, FRQ0 * 8:FRQ0 * 8 + 16], s_[:, FRQ0 * 8:FRQ0 * 8 + 16], AF.Exp, [ks], [ks], scale=-0.5)
                            tt(f8(FRKB), f8(FRK), f8(FB), ALU.mult, [ks], [ks])
                            tt(f8(FRKBG), f8(FRKB), f8(FEG), ALU.mult, [ks], [ks])
                            tt(f8(FRKD), f8(FRK), f8(FER), ALU.mult, [ks], [ks])
                            ts(f8(FNB), f8(FB), -1.0, None, ALU.mult, None, [ks], [ks])
                            ts(f8(FRQ), f8(FRQ0), 128.0 ** -0.5, None, ALU.mult, None, [ks], [ks])
                            tt(f8(FRQE), f8(FRQ), f8(FEG), ALU.mult, [ks], [ks])
                            return s_, ks, zns

                        def sc(s_, f, q, hl):
                            c = col(f, q, hl)
                            return s_[:, c:c + 1]

                        def prep_pre(t, hl, s_, ks, qf_, kqf, res):
                            q = t % 4
                            qk = QKVt[:, q, :]
                            kt_ = qk[:, 256 + hl * 128:256 + (hl + 1) * 128]
                            vt_ = qk[:, 512 + hl * 128:512 + (hl + 1) * 128]
                            kn_, kkn = kn_b.next(); kbg, kkbg = kbg_b.next(); kdec, kkdec = kdec_b.next()
                            vb, kvb = vb_b.next()
                            dg, kdg = dg_b.next()
                            act(kn_[:], kt_, AF.Copy, [kQKV[q], ks], [kkn], scale=sc(s_, FRK, q, hl))
                            ts(dg[:], ident_f[:], sc(s_, FGC, q, hl), None, ALU.mult, None, [tC, ks], [kdg], eng="gpsimd")
                            ts(kbg[:], kt_, sc(s_, FRKBG, q, hl), None, ALU.mult, None, [kQKV[q], ks], [kkbg])
                            act(kdec[:], kt_, AF.Copy, [kQKV[q], ks], [kkdec], scale=sc(s_, FRKD, q, hl))
                            ts(vb[:], vt_, sc(s_, FB, q, hl), None, ALU.mult, None, [kQKV[q], ks], [kvb])
                            yield
                            bB, kB = nb()
                            mm(bB[:, 0:128], ones_f[:], dg[:], [tC, kdg], [kB], start=True, stop=False)
                            mm(bB[:, 0:128], ident_f[:], mneg_f[:], [tC], [kB], start=False, stop=True)
                            yield
                            E, kE = E_b.next(); Es, kEs = Es_b.next()
                            act(E[:], bB[:, 0:128], AF.Exp, [kB, ks], [kE], scale=-1.0, bias=sc(s_, FGC, q, hl))
                            b1_, k1_ = nb()
                            b1v = psv(b1_)
                            tr(b1v[:, 0:128], kn_[:], idn[:], [kkn, tC], [k1_])
                            yield
                            knT, kknT = knT_b.next()
                            cp(knT[:], b1v[:, 0:128], [k1_], [kknT], eng="scalar")
                            tt(Es[:], E[:], sl_b[:], ALU.mult, [kE, tC], [kEs])
                            yield
                            bG, kG = nb()
                            mm(bG[:, 0:128], knT[:], knT[:], [kknT], [kG])
                            mm(bG[:, 128:256], qf_[:, hl, q * 128:(q + 1) * 128], knT[:], [kqf, kknT], [kG])
                            yield
                            A0, kA0 = A_b.next(); at_, kat = at_b.next()
                            stt(A0[:], bG[:, 0:128], sc(s_, FNB, q, hl), Es[:], ALU.mult, ALU.mult, [kG, ks, kEs], [kA0])
                            stt(at_[:], bG[:, 128:256], sc(s_, FRQ, q, hl), E[:], ALU.mult, ALU.mult, [kG, ks, kE], [kat])
                            yield
                            bT, kT = nb()
                            bTv = psv(bT)
                            tr(bTv[:, 0:128], A0[:], idn[:], [kA0, tC], [kT])
                            tr(bTv[:, 128:256], at_[:], idn[:], [kat, tC], [kT])
                            yield
                            B0, kB0 = A_b.next(); atT, katT = atT_b.next()
                            cp(B0[:], bTv[:, 0:128], [kT], [kB0], eng="scalar")
                            cp(atT[:], bTv[:, 128:256], [kT], [katT], eng="vector")
                            yield
                            P0, kP0 = P_b.next()
                            tt(P0[:], B0[:], idn[:], ALU.add, [kB0, tC], [kP0], eng="gpsimd")
                            res.update(dict(A=(A0, kA0), B=(B0, kB0), P=(P0, kP0), kbg=(kbg, kkbg), kdec=(kdec, kkdec),
                                            vb=(vb, kvb), atT=(atT, katT), hl=hl))

                        def solve_sq(sl, lev, flip):
                            (Ac, kAc), (Bc, kBc) = sl["A"], sl["B"]
                            bS, kSb = nb()
                            mm(bS[:, 0:128], Bc[:], Ac[:], [kBc, kAc], [kSb])
                            if lev < 5:
                                mm(bS[:, 128:256], Ac[:], Bc[:], [kBc, kAc], [kSb])
                            An, kAn = A_b.next()
                            e1 = "scalar" if flip else "vector"
                            cp(An[:], bS[:, 0:128], [kSb], [kAn], eng=e1)
                            if lev < 5:
                                Bn, kBn = A_b.next()
                                cp(Bn[:], bS[:, 128:256], [kSb], [kBn], eng=e1)
                            else:
                                Bn, kBn = None, None
                            sl["A"], sl["B"] = (An, kAn), (Bn, kBn)

                        def solve_pu(sl, lev):
                            (An, kAn), (Pc, kPc) = sl["A"], sl["P"]
                            bP, kPb = nb()
                            mm(bP[:, 0:128], An[:], Pc[:], [kAn, kPc], [kPb])
                            Pn, kPn = (P_b.next() if lev < 5 else TT_b.next())
                            tt(Pn[:], bP[:, 0:128], Pc[:], ALU.add, [kPb, kPc], [kPn])
                            sl["P"] = (Pn, kPn)

                        def post_group(slots):
                            for h0 in range(0, len(slots), 4):
                                part = slots[h0:h0 + 4]
                                bks = []
                                for sl in part:
                                    TT, kTT = sl["P"]
                                    kbg, kkbg = sl["kbg"]
                                    bw, kw = nb()
                                    mm(bw[:, 0:128], kbg[:], TT[:], [kkbg, kTT], [kw])
                                    bks.append((bw, kw))
                                for sl, (bw, kw) in zip(part, bks):
                                    nwT, knwT = nwT_b.next()
                                    act(nwT[:], bw[:, 0:128], AF.Copy, [kw], [knwT], scale=-1.0)
                                    sl["nwT"] = (nwT, knwT)

                        def scan_head(t, s_, ks, zn_, kzn, sl, qf_, kqf, yb_, kyb):
                            q = t % 4
                            hl = sl["hl"]
                            TT, kTT = sl["P"]; nwT, knwT = sl["nwT"]
                            kdec, kkdec = sl["kdec"]; vb, kvb = sl["vb"]; atT, katT = sl["atT"]
                            Sh = Sst[:, hl, :]
                            Shb = Sbf[:, hl, :]
                            bV, kVb = nb()
                            mm(bV[:, 0:128], TT[:], vb[:], [kTT, kvb], [kVb], start=True, stop=(t == 0))
                            if t > 0:
                                mm(bV[:, 0:128], nwT[:], Shb, [knwT, kS[hl]], [kVb], start=False, stop=True)
                            yield
                            vnew, kvn = vnew_b.next()
                            cp(vnew[:], bV[:, 0:128], [kVb], [kvn], eng="scalar")
                            yield
                            bO, kO = nb()
                            if t > 0:
                                mm(bO[:, 0:128], qf_[:, hl, q * 128:(q + 1) * 128], Shb, [kqf, kS[hl]], [kO])
                            mm(bO[:, 128:256], atT[:], vnew[:], [katT, kvn], [kO])
                            bS2, kS2 = nb()
                            mm(bS2[:, 0:128], kdec[:], vnew[:], [kkdec, kvn], [kS2])
                            yield
                            if t > 0:
                                if DT_B != F32:
                                    stt(Shb, Sh, sc(s_, FEL, q, hl), bS2[:, 0:128], ALU.mult, ALU.add, [kS[hl], ks, kS2], [kS[hl]])
                                stt(Sh, Sh, sc(s_, FEL, q, hl), bS2[:, 0:128], ALU.mult, ALU.add, [kS[hl], ks, kS2], [kS[hl]])
                            else:
                                if DT_B != F32:
                                    cp(Shb, bS2[:, 0:128], [kS2], [kS[hl]])
                                cp(Sh, bS2[:, 0:128], [kS2], [kS[hl]])
                            o_, ko_ = o_b.next()
                            if t > 0:
                                ot, kot = otmp_b.next()
                                ts(ot[:], bO[:, 0:128], sc(s_, FRQE, q, hl), None, ALU.mult, None, [kO, ks], [kot])
                                tt(o_[:], bO[:, 128:256], ot[:], ALU.add, [kO, kot], [ko_])
                            else:
                                cp(o_[:], bO[:, 128:256], [kO], [ko_])
                            yield
                            jk2, kjk2 = junk_b.next()
                            act(jk2[:], o_[:], AF.Square, [ko_], [kjk2, ks], accum_out=sc(s_, FSSO, q, hl))
                            act(sc(s_, FRST, q, hl), sc(s_, FSSO, q, hl), AF.Ln, [ks], [ks], bias=1e-6, scale=1.0 / 128.0)
                            act(sc(s_, FRST, q, hl), sc(s_, FRST, q, hl), AF.Exp, [ks], [ks], scale=-0.5)
                            yield
                            stt(yb_[:, hl * 128:(hl + 1) * 128], o_[:], sc(s_, FRST, q, hl), zn_[:, hl * 128:(hl + 1) * 128],
                                ALU.mult, ALU.mult, [ko_, ks, kzn], [kyb])

                        def scan_tile(info):
                            t, (s_, ks, zn_, kzn), slots, qf_, kqf = info
                            yb_, kyb = yb.next()
                            run_il([scan_head(t, s_, ks, zn_, kzn, sl, qf_, kqf, yb_, kyb) for sl in slots], 2)
                            yT_, kyT = ybT.next()
                            transpose_bf(yb_, kyb, 2, yT_, kyT)
                            outproj(t, yT_, kyT, WoB, kWo, 2, first_acc[t])
                            first_acc[t] = False

                        prev = None
                        for g in range(NG):
                            qf_, kqf = b1(g)
                            s_, ks, zns = gates_group(g)
                            tiles = []
                            gens = []
                            for q in range(4):
                                t = 4 * g + q
                                gi = (s_, ks, zns[q][0], zns[q][1])
                                sls = [dict(), dict()]
                                for hl in range(2):
                                    gens.append(prep_pre(t, hl, s_, ks, qf_, kqf, sls[hl]))
                                tiles.append((t, gi, sls, qf_, kqf))
                            run_il(gens, 4)
                            allslots = [s for ti in tiles for s in ti[2]]
                            for lev in range(6):
                                for si, s in enumerate(allslots):
                                    solve_sq(s, lev, si % 2)
                                for s in allslots:
                                    solve_pu(s, lev)
                                if prev is not None and lev < 4:
                                    scan_tile(prev[lev])
                            post_group(allslots)
                            prev = tiles
                        for q in range(4):
                            scan_tile(prev[q])
                        S.barrier()

            moe = (l % 2 == 1)
            li = l // 2
            if "F" in steps:
                with contextlib.ExitStack() as st:
                    def al(n, s, d):
                        return st.enter_context(nc.sbuf_tensor(f"F{l}_{n}", s, d))
                    g1 = al("g1", [128, D], F32); b1_ = al("b1", [128, D], F32)
                    g2 = al("g2", [128, D], F32); b2_ = al("b2", [128, D], F32); kLP = Tok()
                    S.dma("sync", g1[:], I["ln1_g"][l:l + 1, :].broadcast_to([128, D]), dw[2], writes=[kLP])
                    S.dma("sync", b1_[:], I["ln1_b"][l:l + 1, :].broadcast_to([128, D]), dw[2], writes=[kLP])
                    S.dma("sync", g2[:], I["ln2_g"][l:l + 1, :].broadcast_to([128, D]), dw[2], writes=[kLP])
                    S.dma("sync", b2_[:], I["ln2_b"][l:l + 1, :].broadcast_to([128, D]), dw[2], writes=[kLP])
                    kLP.w = (dw[2].name, dw[2].count)
                    sm = (al("st6", [128, NT, 2, 6], F32), al("mv", [128, NT, 2], F32), al("sd", [128, NT], F32),
                          al("rs", [128, NT], F32), [Tok() for _ in range(NT)])
                    comb = al("comb", [128, NT, NE], F32); kcomb = [Tok() for _ in range(NT)]
                    if moe:
                        Wr = al("Wr", [128, 8, NE], F32); kWr = Tok()
                        S.dma("sync", Wr[:], I["moe_router"][li].rearrange("(c p) e -> p c e", p=128), dw[2], writes=[kWr])
                        kWr.w = (dw[2].name, dw[2].count)
                        kLP.w = (dw[2].name, dw[2].count)
                        xtf = RB(al, "xtf", 2, [128, 512], F32)
                        lgb = {}
                        rsm = RB(al, "rsm", 2, [128, 64], F32)

                        def router(g, c, bk, bt):
                            if c == 0:
                                lgb["b"] = nb_special()
                            lb, klb = lgb["b"]
                            xf_, kxf = xtf.next()
                            ts(xf_[:], bk[:], 1.0 / ALPHA, None, ALU.mult, None, [bt], [kxf])
                            for q in range(4):
                                mm(lb[:, q * 8:(q + 1) * 8], xf_[:, q * 128:(q + 1) * 128], Wr[:, c, :], [kxf, kWr], [klb],
                                   start=(c == 0 and q == 0), stop=(c == 7), sgc=True)
                            if c == 7:
                                for q in range(4):
                                    t = 4 * g + q
                                    r_, kr_ = rsm.next()
                                    lg = r_[:, 0:8]
                                    cp(lg, lb[:, q * 8:(q + 1) * 8], [klb], [kr_])
                                    S.op("vector", lambda e: e.max(out=r_[:, 8:16], in_=lg), [kr_], [kr_])
                                    ts(r_[:, 16:24], lg, r_[:, 8:9], None, ALU.is_equal, None, [kr_], [kr_])
                                    ts(r_[:, 24:32], lg, r_[:, 9:10], None, ALU.is_equal, None, [kr_], [kr_])
                                    tt(r_[:, 32:33], r_[:, 9:10], r_[:, 8:9], ALU.subtract, [kr_], [kr_])
                                    act(r_[:, 32:33], r_[:, 32:33], AF.Exp, [kr_], [kr_])
                                    ts(r_[:, 33:34], r_[:, 32:33], 1.0, None, ALU.add, None, [kr_], [kr_])
                                    S.op("vector", lambda e: e.reciprocal(out=r_[:, 34:35], in_=r_[:, 33:34]), [kr_], [kr_])
                                    tt(r_[:, 35:36], r_[:, 32:33], r_[:, 34:35], ALU.mult, [kr_], [kr_])
                                    ts(r_[:, 16:24], r_[:, 16:24], r_[:, 34:35], None, ALU.mult, None, [kr_], [kr_])
                                    stt(comb[:, t, :], r_[:, 24:32], r_[:, 35:36], r_[:, 16:24], ALU.mult, ALU.add, [kr_], [kcomb[t]])
                    else:
                        router = None

                    ts(g1[:], g1[:], ALPHA, None, ALU.mult, None, [kLP], [kLP])
                    ts(b1_[:], b1_[:], ALPHA, None, ALU.mult, None, [kLP], [kLP])
                    groups = [(0, 4), (4, 4), (8, 4), (12, 4), (16, 4), (20, 2)]
                    Wg = RB(al, "Wg", 2, [128, 8, 512], BF16)
                    Wu = RB(al, "Wu", 2, [128, 8, 512], BF16)
                    Wd = RB(al, "Wd", 2, [128, 4, D], BF16)
                    actb = al("actb", [128, 4, SEQ], BF16); kact = [Tok() for _ in range(NG)]
                    sgb = RB(al, "sgb", 3, [128, 512], BF16)
                    experts = list(range(NE)) if moe else [None]
                    items = [(ex, c0, ncn) for ex in experts for (c0, ncn) in groups]
                    loaded = {}

                    def load_w(n):
                        if n >= len(items) or n in loaded:
                            return
                        ex, c0, ncn = items[n]
                        if moe:
                            wg_src = I["moe_w_gate"][li, ex]; wu_src = I["moe_w_up"][li, ex]; wd_src = I["moe_w_down"][li, ex]
                        else:
                            wg_src = I["ffn_w_gate"][li]; wu_src = I["ffn_w_up"][li]; wd_src = I["ffn_w_down"][li]
                        wgv = wg_src.rearrange("(c p) f -> p c f", p=128)
                        wuv = wu_src.rearrange("(c p) f -> p c f", p=128)
                        wdv = wd_src.rearrange("(c p) f -> p c f", p=128)
                        sl_i = n % 2
                        wg_, kwg = Wg.next(); wu_, kwu = Wu.next(); wd_, kwd = Wd.next()
                        S.dma("gpsimd", wg_[:, :, 0:ncn * 128], wgv[:, :, c0 * 128:(c0 + ncn) * 128], dw[sl_i], writes=[kwg])
                        S.dma("gpsimd", wu_[:, :, 0:ncn * 128], wuv[:, :, c0 * 128:(c0 + ncn) * 128], dw[sl_i], writes=[kwu])
                        S.dma("gpsimd", wd_[:, 0:ncn, :], wdv[:, c0:c0 + ncn, :], dw[sl_i], writes=[kwd])
                        fin = (dw[sl_i].name, dw[sl_i].count)
                        kwg.w = fin; kwu.w = fin; kwd.w = fin
                        loaded[n] = (wg_, kwg, wu_, kwu, wd_, kwd)

                    load_w(0)
                    load_w(1)

                    for g in range(NG):
                        run_il([ln_gen(4 * g + q, g1, b1_, kLP, sm) for q in range(4)], 4)
                        make_xt(g, router, scale=1.0 / ALPHA)

                    for n, (ex, c0, ncn) in enumerate(items):
                        if True:
                            if True:
                                wg_, kwg, wu_, kwu, wd_, kwd = loaded[n]
                            for g in range(NG):
                                for cc in range(ncn):
                                    pg, kpg = nb()
                                    for c in range(8):
                                        mm(pg[:], wg_[:, c, cc * 128:(cc + 1) * 128], XT[:, c, g * 512:(g + 1) * 512],
                                           [kwg, tXT[g]], [kpg], start=(c == 0), stop=(c == 7))
                                    pu, kpu = nb()
                                    for c in range(8):
                                        mm(pu[:], wu_[:, c, cc * 128:(cc + 1) * 128], XT[:, c, g * 512:(g + 1) * 512],
                                           [kwu, tXT[g]], [kpu], start=(c == 0), stop=(c == 7))
                                    sg_, ksg = sgb.next()
                                    act(sg_[:], pg[:], AF.Silu, [kpg], [ksg])
                                    tt(actb[:, cc, g * 512:(g + 1) * 512], sg_[:], pu[:], ALU.mult, [ksg, kpu], [kact[g]])
                            for t in range(NT):
                                g = t // 4
                                for hf in range(2):
                                    bk, bt = nb()
                                    for cc in range(ncn):
                                        mm(bk[:], actb[:, cc, t * 128:(t + 1) * 128], wd_[:, cc, hf * 512:(hf + 1) * 512],
                                           [kact[g], kwd], [bt], start=(cc == 0), stop=(cc == ncn - 1))
                                    xs = X[:, t, hf * 512:(hf + 1) * 512]
                                    if moe:
                                        stt(xs, bk[:], comb[:, t, ex:ex + 1], xs, ALU.mult, ALU.add, [bt, kcomb[t], tX[t]], [tX[t]])
                                    else:
                                        tt(xs, bk[:], xs, ALU.add, [bt, tX[t]], [tX[t]])
                        load_w(n + 2)
                    last = (l == n_layers - 1)
                    outv = out.rearrange("(t p) d -> p t d", p=128)
                    def store_tile(t):
                        S.dma("sync", outv[:, t, :], X[:, t, :], dout, reads=[tX[t]])
                    def ln2_group(g):
                        run_il([ln_gen(4 * g + q, g2, b2_, kLP, sm, after=(store_tile if last else None))
                                for q in range(4)], 4)
                    for g in range(NG):
                        ln2_group(g)
                        if not last:
                            make_xt(g)
                    S.barrier(rotate=not last)
            elif dbg:
                pass

        if "F" not in steps:
            outv = out.rearrange("(t p) d -> p t d", p=128)
            for t in range(NT):
                S.dma("sync", outv[:, t, :], X[:, t, :], dout, reads=[tX[t]])
        S.wait_events("sync", [(dout.name, dout.count)])
        S.barrier()
        print("instr counts", S.ninst, "gen", S.gen)
    return nc


def prep_inputs(inputs, n_layers=DEPTH):
    f = lambda a: np.ascontiguousarray(np.asarray(a, dtype=np.float32))
    shared = {}
    for k in ("w_in", "a_ln_g", "a_ln_b", "b_a_log", "b_dt_bias", "b_norm_g", "w_out", "ln1_g", "ln1_b", "ln2_g",
              "ln2_b", "ffn_w_gate", "ffn_w_up", "ffn_w_down", "moe_router", "moe_w_gate", "moe_w_up", "moe_w_down"):
        shared[k] = f(inputs[k])
    cw = np.asarray(inputs["conv_w"], np.float32)
    shared["conv_w"] = f(cw.reshape(DEPTH, 4, 12, 128).transpose(0, 3, 2, 1))
    ws = np.asarray(inputs["a_ws"], np.float32)
    shared["a_ws"] = f(ws.transpose(0, 3, 1, 2))
    shared["a_bs"] = f(np.asarray(inputs["a_bs"], np.float32).transpose(0, 2, 1))
    if n_layers < DEPTH:
        for k in list(shared.keys()):
            shp = small_shape(k, IN_SHAPES[k], n_layers)
            sl = tuple(slice(0, n) for n in shp)
            shared[k] = f(shared[k][sl])
    shared.update(host_consts())
    x = np.asarray(inputs["x"], np.float32)
    in_maps = []
    for b in range(8):
        m = dict(shared)
        m["x"] = f(x[b])
        in_maps.append(m)
    return in_maps


def kernel(**inputs):
    in_maps = prep_inputs(inputs)
    nc = build()
    res = run_bass_kernel_spmd(nc, in_maps, core_ids=list(range(8)))
    return np.stack([np.asarray(r["out"], dtype=np.float32) for r in res.results], axis=0)
```

```python
import contextlib
import math
import numpy as np
import concourse.bass as bass
import concourse.mybir as mybir
from concourse.bass_utils import run_bass_kernel_spmd

F32 = mybir.dt.float32
BF16 = mybir.dt.bfloat16
AF = mybir.ActivationFunctionType
ALU = mybir.AluOpType
AX = mybir.AxisListType

D = 1024
SEQ = 2048
NT = 16
NG = 4
DEPTH = 4
D_IN = 3336
D_FF = 2816
NFC = 22
NE = 8
ALPHA = (2.0 * DEPTH) ** 0.25
DT_B = BF16


class Tok:
    __slots__ = ("name", "w", "r", "excl")

    def __init__(self, name="", excl=False):
        self.name = name
        self.w = None
        self.r = []
        self.excl = excl


class DSem:
    def __init__(self, name, handle):
        self.name = name
        self.handle = handle
        self.count = 0


class Sched:
    ENG = ("tensor", "vector", "scalar", "gpsimd", "sync")

    def __init__(self, nc, stack):
        self.nc = nc
        self.stack = stack
        self.gen = 0
        self.handles = {}
        self.retired = set()
        self.dsems = []
        self.ninst = {e: 0 for e in self.ENG}
        self._new_sems()

    def _new_sems(self):
        self.key = {}
        self.cnt = {}
        for e in self.ENG:
            k = f"{e}#{self.gen}"
            self.key[e] = k
            self.handles[k] = self.stack.enter_context(self.nc.semaphore(f"s_{e}_{self.gen}"))
            self.cnt[e] = 0
        self.waited = {e: {} for e in self.ENG}

    def dsem(self, name):
        h = self.stack.enter_context(self.nc.semaphore("d_" + name))
        ds = DSem("d_" + name, h)
        self.handles[ds.name] = h
        self.dsems.append(ds)
        return ds

    def _waits(self, e, reads, writes):
        need = {}
        mykey = self.key[e]

        def req(ev, same_ok):
            key, val = ev
            if key in self.retired:
                return
            if key == mykey and same_ok and e == "tensor":
                return
            if need.get(key, 0) < val:
                need[key] = val

        for t in reads:
            if t.w is not None:
                req(t.w, False)
            if t.excl:
                for ev in t.r:
                    if ev[0] != mykey:
                        req(ev, False)
        for t in writes:
            if t.w is not None:
                req(t.w, True)
            for ev in t.r:
                req(ev, True)
        out = []
        w = self.waited[e]
        for key, val in need.items():
            if w.get(key, 0) < val:
                w[key] = val
                out.append((self.handles[key], val))
        return out

    def _emit(self, e, waits, fn, sem, inc):
        eng = getattr(self.nc, e)
        for h, val in waits:
            eng.wait_ge(h, val)
            self.ninst[e] += 1
        if fn is not None:
            fn(eng).then_inc(sem, inc)
            self.ninst[e] += 1

    def op(self, e, fn, reads=(), writes=()):
        waits = self._waits(e, reads, writes)
        self.cnt[e] += 1
        ev = (self.key[e], self.cnt[e])
        self._emit(e, waits, fn, self.handles[self.key[e]], 1)
        for t in reads:
            t.r.append(ev)
            if len(t.r) > 32:
                mx = {}
                for k_, v_ in t.r:
                    if k_ not in self.retired and mx.get(k_, 0) < v_:
                        mx[k_] = v_
                t.r = list(mx.items())
        for t in writes:
            t.w = ev
            t.r = []
        return ev

    def dma(self, q, out, in_, ds, reads=(), writes=(), **kw):
        waits = self._waits(q, reads, writes)
        ds.count += 16
        ev = (ds.name, ds.count)
        self._emit(q, waits, lambda eng: eng.dma_start(out=out, in_=in_, **kw), ds.handle, 16)
        for t in reads:
            t.r.append(ev)
        for t in writes:
            t.w = ev
            t.r = []
        return ev

    def wait_events(self, e, events):
        tok = Tok("w")
        for ev in events:
            tok.w = ev
            waits = self._waits(e, [tok], [])
            self._emit(e, waits, None, None, 0)

    def barrier(self, rotate=False):
        evs = [(self.key[e], self.cnt[e]) for e in self.ENG if self.cnt[e] > 0]
        evs += [(ds.name, ds.count) for ds in self.dsems if ds.count > 0]
        for e in self.ENG:
            self.wait_events(e, evs)
        if rotate:
            for e in self.ENG:
                self.retired.add(self.key[e])
            self.gen += 1
            self._new_sems()
            for e in self.ENG:
                for ds in self.dsems:
                    self.waited[e][ds.name] = ds.count


def run_il(gens, width):
    it = iter(gens)
    active = []
    while True:
        while len(active) < width:
            try:
                active.append(next(it))
            except StopIteration:
                break
        if not active:
            break
        nxt = []
        for gn in active:
            try:
                next(gn)
                nxt.append(gn)
            except StopIteration:
                pass
        active = nxt


class RB:
    def __init__(self, alloc, name, n, shape, dt):
        self.t = [alloc(f"{name}{i}", shape, dt) for i in range(n)]
        self.k = [Tok(f"{name}{i}") for i in range(n)]
        self.i = 0

    def next(self):
        i = self.i
        self.i = (i + 1) % len(self.t)
        return self.t[i], self.k[i]


def host_consts():
    c = {}
    i = np.arange(128)
    c["ident"] = np.eye(128, dtype=np.float32)
    c["trilT"] = (i[:, None] <= i[None, :]).astype(np.float32)
    c["ones"] = np.ones((128, 128), np.float32)
    c["mneg"] = np.where(i[None, :] <= i[:, None], 0.0, 30000.0).astype(np.float32)
    c["sl"] = (i[None, :] < i[:, None]).astype(np.float32)
    inv = 10000.0 ** (-np.arange(0, 64, 2, dtype=np.float32) / 64.0)
    ang = np.arange(SEQ, dtype=np.float32)[:, None] * inv[None, :]
    c["cos"] = np.ascontiguousarray(np.cos(ang).astype(np.float32).reshape(16, 128, 32).transpose(1, 0, 2))
    c["sin"] = np.ascontiguousarray(np.sin(ang).astype(np.float32).reshape(16, 128, 32).transpose(1, 0, 2))
    u = np.arange(SEQ)[None, :] - i[:, None]
    m = ((u >= 0) & (u <= 128)).astype(np.float32)
    m += ((u >= 0) & (u <= 512) & (u % 4 == 0)).astype(np.float32)
    m += ((u >= 0) & (u <= 2048) & (u % 16 == 0)).astype(np.float32)
    c["mstrip"] = m.astype(np.float32)
    return c


CONST_SHAPES = {"ident": [128, 128], "trilT": [128, 128], "ones": [128, 128], "mneg": [128, 128],
                "sl": [128, 128], "cos": [128, 16, 32], "sin": [128, 16, 32], "mstrip": [128, SEQ]}

IN_SHAPES = {
    "x": [SEQ, D], "w_in": [DEPTH, D, D_IN], "conv_w": [DEPTH, 128, 12, 4], "a_ln_g": [DEPTH, 256],
    "a_ln_b": [DEPTH, 256], "a_ws": [DEPTH, 128, 4, 128], "a_bs": [DEPTH, 128, 4], "b_a_log": [DEPTH, 4],
    "b_dt_bias": [DEPTH, 4], "b_norm_g": [DEPTH, 128], "w_out": [DEPTH, D, D], "ln1_g": [DEPTH, D],
    "ln1_b": [DEPTH, D], "ln2_g": [DEPTH, D], "ln2_b": [DEPTH, D], "ffn_w_gate": [2, D, D_FF],
    "ffn_w_up": [2, D, D_FF], "ffn_w_down": [2, D_FF, D], "moe_router": [2, D, NE],
    "moe_w_gate": [2, NE, D, D_FF], "moe_w_up": [2, NE, D, D_FF], "moe_w_down": [2, NE, D_FF, D],
}


import os
KDBG = os.environ.get("KDBG", "")


def small_shape(k, shp, n_layers):
    if n_layers >= DEPTH or k == "x":
        return list(shp)
    shp = list(shp)
    if k.startswith("ffn_"):
        shp[0] = (n_layers + 1) // 2
        if "nof" in KDBG:
            shp = [1, 8, 8]
    elif k.startswith("moe_"):
        shp[0] = max(1, n_layers // 2)
        if n_layers < 2:
            shp = [1] + [1] * (len(shp) - 3) + shp[-2:] if len(shp) == 4 else shp
    else:
        shp[0] = n_layers
    return shp


def build(n_layers=DEPTH, steps="ACBF", dbg=False):
    nc = bass.Bass("TRN2", target_bir_lowering=False)
    I = {}
    for k, shp in list(IN_SHAPES.items()):
        I[k] = nc.dram_tensor(k, small_shape(k, shp, n_layers), F32, kind="ExternalInput").ap()
    for k, shp in list(CONST_SHAPES.items()):
        I[k] = nc.dram_tensor(k, shp, F32, kind="ExternalInput").ap()
    out = nc.dram_tensor("out", [SEQ, D], F32, kind="ExternalOutput").ap()

    with contextlib.ExitStack() as gst:
        S = Sched(nc, gst)

        def galloc(n, s, d):
            return gst.enter_context(nc.sbuf_tensor(n, s, d))

        X = galloc("X", [128, NT, D], F32)
        XT = galloc("XT", [128, 8, SEQ], BF16)
        tX = [Tok(f"X{t}") for t in range(NT)]
        tXT = [Tok(f"XT{g}") for g in range(NG)]
        ident_f = galloc("ident_f", [128, 128], F32)
        ident_b = galloc("ident_b", [128, 128], BF16)
        trilT_f = galloc("trilT_f", [128, 128], F32)
        ones_f = galloc("ones_f", [128, 128], F32)
        mneg_f = galloc("mneg_f", [128, 128], F32)
        sl_b = galloc("sl_b", [128, 128], DT_B)
        tC = Tok("consts")
        banks = [gst.enter_context(nc.psum_tensor(f"bank{i}", [128, 512], F32)) for i in range(8)]
        bank_tok = [Tok(f"bank{i}", excl=True) for i in range(8)]
        bstate = {"i": 0}

        def nb():
            i = bstate["i"]
            bstate["i"] = (i + 1) % 6
            return banks[i], bank_tok[i]

        def nb_special():
            i = 6 + bstate.get("s", 0)
            bstate["s"] = bstate.get("s", 0) ^ 1
            return banks[i], bank_tok[i]

        def mm(o, lhsT, rhs, r, w, start=True, stop=True, sgc=False):
            S.op("tensor", lambda e: e.matmul(o, lhsT=lhsT, rhs=rhs, start=start, stop=stop, skip_group_check=sgc), r, w)

        def tr(o, i_, idn, r, w):
            S.op("tensor", lambda e: e.transpose(out=o, in_=i_, identity=idn), r, w)

        def act(o, i_, func, r, w, **kw):
            S.op("scalar", lambda e: e.activation(out=o, in_=i_, func=func, **kw), r, w)

        def ts(o, i0, s1, s2, op0, op1, r, w, eng="vector"):
            if op1 is None:
                S.op(eng, lambda e: e.tensor_scalar(out=o, in0=i0, scalar1=s1, scalar2=None, op0=op0), r, w)
            else:
                S.op(eng, lambda e: e.tensor_scalar(out=o, in0=i0, scalar1=s1, scalar2=s2, op0=op0, op1=op1), r, w)

        def tt(o, i0, i1, op, r, w, eng="vector"):
            S.op(eng, lambda e: e.tensor_tensor(out=o, in0=i0, in1=i1, op=op), r, w)

        def stt(o, i0, sc, i1, op0, op1, r, w):
            S.op("vector", lambda e: e.scalar_tensor_tensor(out=o, in0=i0, scalar=sc, in1=i1, op0=op0, op1=op1), r, w)

        def cp(o, i_, r, w, eng="vector"):
            if eng == "scalar":
                S.op("scalar", lambda e: e.copy(out=o, in_=i_), r, w)
            else:
                S.op(eng, lambda e: e.tensor_copy(out=o, in_=i_), r, w)

        dconst = S.dsem("const")
        dx = S.dsem("x")
        dw = [S.dsem(f"w{i}") for i in range(6)]
        dout = S.dsem("out")

        with contextlib.ExitStack() as st0:
            sl_f = st0.enter_context(nc.sbuf_tensor("sl_f", [128, 128], F32))
            tmpk = Tok()
            S.dma("sync", ident_f[:], I["ident"], dconst, writes=[tC])
            S.dma("sync", trilT_f[:], I["trilT"], dconst, writes=[tC])
            S.dma("sync", ones_f[:], I["ones"], dconst, writes=[tC])
            S.dma("sync", mneg_f[:], I["mneg"], dconst, writes=[tC])
            S.dma("sync", sl_f[:], I["sl"], dconst, writes=[tmpk])
            tC.w = (dconst.name, dconst.count)
            tmpk.w = (dconst.name, dconst.count)
            cp(ident_b[:], ident_f[:], [tC], [tC])
            cp(sl_b[:], sl_f[:], [tmpk], [tC])
            xv = I["x"].rearrange("(t p) d -> p t d", p=128)
            for q in range(4):
                S.dma("sync" if q % 2 == 0 else "scalar", X[:, 4 * q:4 * q + 4, :], xv[:, 4 * q:4 * q + 4, :], dx,
                      writes=tX[4 * q:4 * q + 4])
            for t in range(NT):
                tX[t].w = (dx.name, dx.count)
            S.barrier()

        def make_xt(g, router=None, scale=None):
            for c in range(8):
                bk, bt = nb()
                for q in range(4):
                    t = 4 * g + q
                    tr(bk[:, q * 128:(q + 1) * 128], X[:, t, c * 128:(c + 1) * 128], ident_f[:], [tX[t], tC], [bt])
                dst = XT[:, c, g * 512:(g + 1) * 512]
                if scale is None:
                    cp(dst, bk[:], [bt], [tXT[g]], eng=("scalar" if c % 2 == 0 else "vector"))
                elif c % 2 == 0:
                    act(dst, bk[:], AF.Copy, [bt], [tXT[g]], scale=scale)
                else:
                    ts(dst, bk[:], scale, None, ALU.mult, None, [bt], [tXT[g]])
                if router is not None:
                    router(g, c, bk, bt)

        def ln_gen(t, gam, bet, tpar, sm, eps=1e-5, after=None):
            st6, mv, sd, rs, ksm = sm
            for hf in range(2):
                S.op("vector", lambda e: e.bn_stats(out=st6[:, t, hf, :], in_=X[:, t, hf * 512:(hf + 1) * 512]),
                     [tX[t]], [ksm[t]])
            S.op("vector", lambda e: e.bn_aggr(out=mv[:, t, :], in_=st6[:, t, :, :]), [ksm[t]], [ksm[t]])
            yield
            act(sd[:, t:t + 1], mv[:, t, 1:2], AF.Sqrt, [ksm[t]], [ksm[t]], bias=eps, scale=1.0)
            yield
            S.op("vector", lambda e: e.reciprocal(out=rs[:, t:t + 1], in_=sd[:, t:t + 1]), [ksm[t]], [ksm[t]])
            ts(sd[:, t:t + 1], mv[:, t, 0:1], -1.0, rs[:, t:t + 1], ALU.mult, ALU.mult, [ksm[t]], [ksm[t]])
            yield
            act(X[:, t, :], X[:, t, :], AF.Identity, [tX[t], ksm[t]], [tX[t]], scale=rs[:, t:t + 1], bias=sd[:, t:t + 1])
            yield
            tt(X[:, t, :], X[:, t, :], gam[:], ALU.mult, [tX[t], tpar], [tX[t]])
            yield
            tt(X[:, t, :], X[:, t, :], bet[:], ALU.add, [tX[t], tpar], [tX[t]], eng="gpsimd")
            if after is not None:
                yield
                after(t)

        def accum_X(t, hf, bk, bt, first, cscal=None):
            xs = X[:, t, hf * 512:(hf + 1) * 512]
            if first:
                stt(xs, xs, ALPHA, bk[:], ALU.mult, ALU.add, [tX[t], bt], [tX[t]])
            elif cscal is not None:
                stt(xs, bk[:], cscal, xs, ALU.mult, ALU.add, [tX[t], bt], [tX[t]])
            else:
                tt(xs, bk[:], xs, ALU.add, [tX[t], bt], [tX[t]])

        def outproj(t, yT, kyT, Wo, kWo, nk, first):
            for hf in range(2):
                bk, bt = nb()
                for k in range(nk):
                    mm(bk[:], yT[:, k, :], Wo[:, k, hf * 512:(hf + 1) * 512], [kyT, kWo], [bt],
                       start=(k == 0), stop=(k == nk - 1))
                accum_X(t, hf, bk, bt, first)

        def transpose_bf(src, ksrc, nk, dst, kdst, eng="scalar"):
            bk, bt = nb()
            bkb = bk[:].bitcast(BF16)
            for k in range(nk):
                tr(bkb[:, k * 128:(k + 1) * 128], src[:, k * 128:(k + 1) * 128], ident_b[:], [ksrc, tC], [bt])
            cp(dst[:], bkb[:, 0:nk * 128].rearrange("p (k c) -> p k c", k=nk), [bt], [kdst], eng=eng)

        for g in range(NG):
            make_xt(g)

        for l in range(n_layers):
            winv = I["w_in"][l].rearrange("(c p) f -> p c f", p=128)
            woutv = I["w_out"][l].rearrange("(c p) f -> p c f", p=128)
            first_acc = [True] * NT

            if "A" in steps:
                with contextlib.ExitStack() as st:
                    def al(n, s, d):
                        return st.enter_context(nc.sbuf_tensor(f"A{l}_{n}", s, d))
                    WA = al("WA", [128, 8, 512], BF16); kWA = Tok()
                    WoA = al("WoA", [128, 2, D], BF16); kWo = Tok()
                    Wsf = al("Wsf", [128, 4, 128], F32)
                    WsT = al("WsT", [128, 4, 128], BF16); kWs = Tok()
                    bsA = al("bsA", [128, 4], F32)
                    lng = al("lng", [128, 256], F32)
                    lnb = al("lnb", [128, 256], F32); kP = Tok()
                    S.dma("gpsimd", WA[:], winv[:, :, 0:512], dw[0], writes=[kWA])
                    S.dma("gpsimd", WoA[:], woutv[:, 0:2, :], dw[1], writes=[kWo])
                    S.dma("sync", Wsf[:], I["a_ws"][l], dw[2], writes=[kWs])
                    S.dma("sync", bsA[:], I["a_bs"][l], dw[2], writes=[kP])
                    S.dma("sync", lng[:], I["a_ln_g"][l:l + 1, :].broadcast_to([128, 256]), dw[2], writes=[kP])
                    S.dma("sync", lnb[:], I["a_ln_b"][l:l + 1, :].broadcast_to([128, 256]), dw[2], writes=[kP])
                    kWs.w = (dw[2].name, dw[2].count)
                    kP.w = (dw[2].name, dw[2].count)
                    tt(WsT[:], Wsf[:], trilT_f[:].unsqueeze(1).broadcast_to([128, 4, 128]), ALU.mult, [kWs, tC], [kWs])
                    WIL = 4
                    sq = RB(al, "sq", WIL, [128, 512], F32)
                    inn = RB(al, "inn", WIL, [128, 512], F32)
                    ge = RB(al, "ge", WIL, [128, 512], F32)
                    vn = RB(al, "vn", WIL, [128, 256], BF16)
                    ya = RB(al, "ya", WIL, [128, 256], BF16)
                    yaT = RB(al, "yaT", WIL, [128, 2, 128], BF16)
                    sm = RB(al, "smA", WIL, [128, 16], F32)

                    def a_tile(t):
                        g = t // 4
                        p1, k1 = nb()
                        for c in range(8):
                            mm(p1[:], XT[:, c, t * 128:(t + 1) * 128], WA[:, c, :], [tXT[g], kWA], [k1],
                               start=(c == 0), stop=(c == 7))
                        sq_, ksq = sq.next(); in_, kin = inn.next(); ge_, kge = ge.next()
                        yield
                        act(sq_[:], p1[:], AF.Square, [k1], [ksq])
                        yield
                        ts(in_[:], sq_[:], 0.044715, 1.0, ALU.mult, ALU.add, [ksq], [kin])
                        tt(in_[:], in_[:], p1[:], ALU.mult, [kin, k1], [kin])
                        yield
                        act(sq_[:], in_[:], AF.Sigmoid, [kin], [ksq], scale=1.5957691216057308)
                        yield
                        tt(ge_[:], sq_[:], p1[:], ALU.mult, [ksq, k1], [kge])
                        s_, ks = sm.next()
                        S.op("vector", lambda e: e.bn_stats(out=s_[:, 0:6], in_=ge_[:, 256:512]), [kge], [ks])
                        S.op("vector", lambda e: e.bn_aggr(out=s_[:, 6:8], in_=s_[:, 0:6]), [ks], [ks])
                        yield
                        act(s_[:, 8:9], s_[:, 7:8], AF.Sqrt, [ks], [ks], bias=1e-5, scale=1.0)
                        yield
                        S.op("vector", lambda e: e.reciprocal(out=s_[:, 9:10], in_=s_[:, 8:9]), [ks], [ks])
                        ts(in_[:, 0:256], ge_[:, 256:512], s_[:, 6:7], s_[:, 9:10], ALU.subtract, ALU.mult,
                           [kge, ks], [kin])
                        yield
                        tt(in_[:, 0:256], in_[:, 0:256], lng[:], ALU.mult, [kin, kP], [kin], eng="gpsimd")
                        vn_, kvn = vn.next()
                        tt(vn_[:], in_[:, 0:256], lnb[:], ALU.add, [kin, kP], [kvn], eng="gpsimd")
                        yield
                        p2, k2 = nb()
                        for gi in range(4):
                            mm(p2[:, gi * 64:(gi + 1) * 64], WsT[:, gi, :], vn_[:, gi * 64:(gi + 1) * 64], [kWs, kvn], [k2])
                        yield
                        ya_, kya = ya.next()
                        for gi in range(4):
                            stt(ya_[:, gi * 64:(gi + 1) * 64], p2[:, gi * 64:(gi + 1) * 64], bsA[:, gi:gi + 1],
                                ge_[:, gi * 64:(gi + 1) * 64], ALU.add, ALU.mult, [k2, kP, kge], [kya])
                        yield
                        yT_, kyT = yaT.next()
                        transpose_bf(ya_, kya, 2, yT_, kyT)
                        yield
                        outproj(t, yT_, kyT, WoA, kWo, 2, first_acc[t])
                        first_acc[t] = False

                    run_il([a_tile(t) for t in range(NT)], WIL)
                    S.barrier()

            if "C" in steps:
                with contextlib.ExitStack() as st:
                    def al(n, s, d):
                        return st.enter_context(nc.sbuf_tensor(f"C{l}_{n}", s, d))
                    WC = al("WC", [128, 8, 768], BF16); kWC = Tok()
                    WoC = al("WoC", [128, 2, D], BF16); kWo = Tok()
                    QT = al("QT", [128, 2, SEQ], BF16); kQT = [Tok() for _ in range(NT)]
                    KT = al("KT", [128, 2, SEQ], BF16); kKT = [Tok() for _ in range(NT)]
                    Vp = al("Vp", [128, NT, 4, 65], BF16); kV = [Tok() for _ in range(NT)]
                    Mst = al("Mst", [128, SEQ], BF16); kM = Tok()
                    cosT = al("cosT", [128, NT, 32], F32)
                    sinT = al("sinT", [128, NT, 32], F32); kcs = Tok()
                    S.dma("gpsimd", WC[:], winv[:, :, 2568:3336], dw[0], writes=[kWC])
                    S.dma("gpsimd", WoC[:], woutv[:, 6:8, :], dw[1], writes=[kWo])
                    S.dma("gpsimd", Mst[:, 0:1024], I["mstrip"][:, 0:1024], dw[3], writes=[kM])
                    S.dma("gpsimd", Mst[:, 1024:2048], I["mstrip"][:, 1024:2048], dw[3], writes=[kM])
                    kM.w = (dw[3].name, dw[3].count)
                    S.dma("sync", cosT[:], I["cos"], dw[2], writes=[kcs])
                    S.dma("sync", sinT[:], I["sin"], dw[2], writes=[kcs])
                    kcs.w = (dw[2].name, dw[2].count)
                    kVall = Tok()
                    S.op("gpsimd", lambda e: e.memset(Vp[:], 1.0), [], [kVall])
                    for t in range(NT):
                        kV[t].w = kVall.w
                    qf = RB(al, "qf", 4, [128, 256], F32)
                    kf = RB(al, "kf", 4, [128, 256], F32)
                    r1 = RB(al, "r1", 8, [128, 4, 32], F32)
                    r2 = RB(al, "r2", 8, [128, 4, 32], F32)
                    r3 = RB(al, "r3", 8, [128, 4, 32], F32)
                    r4 = RB(al, "r4", 8, [128, 4, 32], F32)
                    qr = RB(al, "qr", 4, [128, 256], BF16)
                    kr = RB(al, "kr", 4, [128, 256], BF16)
                    Pb = RB(al, "Pb", 6, [128, 512], BF16)
                    YC = RB(al, "YC", 2, [128, 4, 256], BF16)
                    ycT = RB(al, "ycT", 2, [128, 2, 128], BF16)
                    rc = RB(al, "rc", 4, [128, 4], F32)

                    def rope(src, ksrc, dst, kdst, t):
                        x4 = src[:].rearrange("p (h a i) -> p h a i", h=4, a=2)
                        o4 = dst[:].rearrange("p (h a i) -> p h a i", h=4, a=2)
                        x1 = x4[:, :, 0, :]; x2 = x4[:, :, 1, :]
                        cb = cosT[:, t:t + 1, :].broadcast_to([128, 4, 32])
                        sb_ = sinT[:, t:t + 1, :].broadcast_to([128, 4, 32])
                        a_, ka = r1.next(); b_, kb = r2.next(); c_, kc = r3.next(); d_, kd = r4.next()
                        tt(a_[:], x1, cb, ALU.mult, [ksrc, kcs], [ka])
                        tt(b_[:], x2, sb_, ALU.mult, [ksrc, kcs], [kb])
                        tt(c_[:], x2, cb, ALU.mult, [ksrc, kcs], [kc], eng="gpsimd")
                        tt(d_[:], x1, sb_, ALU.mult, [ksrc, kcs], [kd], eng="gpsimd")
                        tt(o4[:, :, 0, :], a_[:], b_[:], ALU.subtract, [ka, kb], [kdst])
                        tt(o4[:, :, 1, :], c_[:], d_[:], ALU.add, [kc, kd], [kdst])

                    def c_proj(t):
                        g = t // 4
                        pq, kpq = nb()
                        for c in range(8):
                            mm(pq[:, 0:256], XT[:, c, t * 128:(t + 1) * 128], WC[:, c, 0:256], [tXT[g], kWC], [kpq],
                               start=(c == 0), stop=(c == 7))
                        pkv, kpkv = nb()
                        for c in range(8):
                            mm(pkv[:], XT[:, c, t * 128:(t + 1) * 128], WC[:, c, 256:768], [tXT[g], kWC], [kpkv],
                               start=(c == 0), stop=(c == 7))
                        qf_, kqf = qf.next(); kf_, kkf = kf.next()
                        yield
                        act(qf_[:], pq[:, 0:256], AF.Copy, [kpq], [kqf], scale=0.125)
                        act(kf_[:], pkv[:, 0:256], AF.Copy, [kpkv], [kkf])
                        cp(Vp[:, t, :, 0:64], pkv[:, 256:512].rearrange("p (h d) -> p h d", h=4), [kpkv], [kV[t]])
                        qr_, kqr = qr.next(); kr_, kkr = kr.next()
                        yield
                        rope(qf_, kqf, qr_, kqr, t)
                        yield
                        rope(kf_, kkf, kr_, kkr, t)
                        yield
                        bk, bt = nb()
                        bkb = bk[:].bitcast(BF16)
                        for k in range(2):
                            tr(bkb[:, k * 128:(k + 1) * 128], qr_[:, k * 128:(k + 1) * 128], ident_b[:], [kqr, tC], [bt])
                        for k in range(2):
                            tr(bkb[:, 256 + k * 128:256 + (k + 1) * 128], kr_[:, k * 128:(k + 1) * 128], ident_b[:],
                               [kkr, tC], [bt])
                        yield
                        cp(QT[:, :, t * 128:(t + 1) * 128], bkb[:, 0:256].rearrange("p (k c) -> p k c", k=2), [bt],
                           [kQT[t]], eng="scalar")
                        cp(KT[:, :, t * 128:(t + 1) * 128], bkb[:, 256:512].rearrange("p (k c) -> p k c", k=2), [bt],
                           [kKT[t]], eng="vector")

                    mask_flip = [0]

                    def attn_head(g, h, yc_, kyc):
                        hp = h // 2; hb = 64 * (h % 2)
                        ob, ko = nb_special()
                        ov = ob[:, 0:260].rearrange("p (i e) -> p i e", e=65)
                        nj = 4 * g + 4

                        def score(j):
                            i0 = max(j, 4 * g)
                            n = (4 * g + 4 - i0) * 128
                            sc, ksc = nb()
                            mm(sc[:, 0:n], KT[hb:hb + 64, hp, j * 128:(j + 1) * 128],
                               QT[hb:hb + 64, hp, i0 * 128:(4 * g + 4) * 128],
                               [kKT[j]] + kQT[i0:4 * g + 4], [ksc])
                            return sc, ksc, i0, n

                        firstmm = True
                        pend = score(0)
                        for j in range(nj):
                            sc, ksc, i0, n = pend
                            p_, kp = Pb.next()
                            act(p_[:, 0:n], sc[:, 0:n], AF.Exp, [ksc], [kp])
                            if j + 1 < nj:
                                pend = score(j + 1)
                            yield
                            u0 = 128 * (i0 - j)
                            mask_flip[0] = (mask_flip[0] + 1) % 3
                            tt(p_[:, 0:n], p_[:, 0:n], Mst[:, u0:u0 + n], ALU.mult, [kp, kM], [kp],
                               eng=("gpsimd" if mask_flip[0] == 0 else "vector"))
                            yield
                            for i in range(i0, 4 * g + 4):
                                mm(ov[:, i - 4 * g, :], p_[:, (i - i0) * 128:(i - i0 + 1) * 128], Vp[:, j, h, :],
                                   [kp, kV[j]], [ko], start=firstmm, stop=(j == i), sgc=True)
                                firstmm = False
                            yield
                        rc_, krc = rc.next()
                        S.op("vector", lambda e: e.reciprocal(out=rc_[:], in_=ov[:, :, 64]), [ko], [krc])
                        for i in range(4):
                            ts(yc_[:, i, h * 64:(h + 1) * 64], ov[:, i, 0:64], rc_[:, i:i + 1], None, ALU.mult, None,
                               [ko, krc], [kyc])

                    def attn_group(g):
                        yc_, kyc = YC.next()
                        if "Cnoh1" in KDBG:
                            run_il([attn_head(g, h, yc_, kyc) for h in (0, 2)], 2)
                        else:
                            run_il([attn_head(g, h, yc_, kyc) for h in range(4)], 2)
                        for i in range(4):
                            t = 4 * g + i
                            yT_, kyT = ycT.next()
                            transpose_bf(yc_[:, i, :], kyc, 2, yT_, kyT)
                            outproj(t, yT_, kyT, WoC, kWo, 2, first_acc[t])
                            first_acc[t] = False

                    for g in range(NG):
                        run_il([c_proj(4 * g + q) for q in range(4)], 2)
                        if "Cproj" not in KDBG:
                            attn_group(g)
                    S.barrier()

            if "B" in steps:
                for hp2 in range(2):
                    with contextlib.ExitStack() as st:
                        def al(n, s, d):
                            return st.enter_context(nc.sbuf_tensor(f"B{l}{hp2}_{n}", s, d))
                        idn = ident_f if DT_B == F32 else ident_b
                        Wq = al("Wq", [128, 8, 768], BF16); kWq = Tok()
                        Wz = al("Wz", [128, 8, 260], BF16); kWz = Tok()
                        WoB = al("WoB", [128, 2, D], BF16); kWo = Tok()
                        cw = al("cw", [128, 12, 4], F32)
                        ngt = al("ngt", [128, 128], F32)
                        alog = al("alog", [128, 4], F32)
                        dtb = al("dtb", [128, 4], F32)
                        nexpA = al("nexpA", [128, 4], F32); kP = Tok()
                        for pi, base in enumerate((512, 1024, 1536)):
                            S.dma("gpsimd", Wq[:, :, pi * 256:(pi + 1) * 256],
                                  winv[:, :, base + hp2 * 256: base + (hp2 + 1) * 256], dw[0], writes=[kWq])
                        kWq.w = (dw[0].name, dw[0].count)
                        S.dma("gpsimd", Wz[:, :, 0:256], winv[:, :, 2048 + hp2 * 256:2048 + (hp2 + 1) * 256], dw[1], writes=[kWz])
                        S.dma("gpsimd", Wz[:, :, 256:258], winv[:, :, 2560 + 2 * hp2:2562 + 2 * hp2], dw[1], writes=[kWz])
                        S.dma("gpsimd", Wz[:, :, 258:260], winv[:, :, 2564 + 2 * hp2:2566 + 2 * hp2], dw[1], writes=[kWz])
                        kWz.w = (dw[1].name, dw[1].count)
                        S.dma("gpsimd", WoB[:], woutv[:, 2 + 2 * hp2:4 + 2 * hp2, :], dw[3], writes=[kWo])
                        S.dma("sync", cw[:], I["conv_w"][l], dw[2], writes=[kP])
                        S.dma("sync", ngt[:], I["b_norm_g"][l:l + 1, :].broadcast_to([128, 128]), dw[2], writes=[kP])
                        S.dma("sync", alog[:], I["b_a_log"][l:l + 1, :].broadcast_to([128, 4]), dw[2], writes=[kP])
                        S.dma("sync", dtb[:], I["b_dt_bias"][l:l + 1, :].broadcast_to([128, 4]), dw[2], writes=[kP])
                        kP.w = (dw[2].name, dw[2].count)
                        act(nexpA[:], alog[:], AF.Exp, [kP], [kP])
                        ts(nexpA[:], nexpA[:], -1.0, None, ALU.mult, None, [kP], [kP])

                        NCH = 4
                        NS = 2 * NCH
                        halo = al("halo", [128, 6, 3], F32); khalo = [Tok() for _ in range(6)]
                        S.op("gpsimd", lambda e: e.memset(halo[:], 0.0), [], khalo)
                        B1W = 2
                        Hb = RB(al, "Hb", B1W, [128, 515], F32)
                        cacc = RB(al, "cacc", B1W, [128, 512], F32)
                        QfT = RB(al, "QfT", 2, [128, 2, 512], DT_B)
                        tmpS = RB(al, "tmpS", B1W, [128, 512], DT_B)
                        QKVt = al("QKVt", [128, 4, 768], DT_B); kQKV = [Tok() for _ in range(4)]
                        Sst = al("Sst", [128, 2, 128], F32); kS = [Tok(), Tok()]
                        if DT_B != F32:
                            Sbf = al("Sbf", [128, 2, 128], DT_B)
                        else:
                            Sbf = Sst
                        zng = RB(al, "zng", 2 * NCH, [128, 256], BF16)
                        zs = RB(al, "zs", 2, [128, 256], F32)
                        gsm = RB(al, "gsm", 3, [128, 160], F32)
                        sqj = RB(al, "sqj", 2, [128, 512], BF16)
                        yb = RB(al, "yb", 2, [128, 256], BF16)
                        ybT = RB(al, "ybT", 2, [128, 2, 128], BF16)

                        def mk(nm, n, dt=DT_B):
                            return RB(al, nm, n, [128, 128], dt)
                        kn_b = mk("kn", NS); knT_b = mk("knT", NS); E_b = mk("E", NS); Es_b = mk("Es", NS)
                        at_b = mk("attn", NS); kbg_b = mk("kbg", NS); dg_b = mk("dg", 8, F32)
                        A_b = mk("Ab", 4 * NS); P_b = mk("Pb", 2 * NS); TT_b = mk("TTb", 2 * NS)
                        kdec_b = mk("kdec", 2 * NS); vb_b = mk("vb", 2 * NS); atT_b = mk("attnT", 2 * NS); nwT_b = mk("nwT", 2 * NS)
                        vnew_b = mk("vnew", 4); o_b = mk("o", 3, F32); otmp_b = mk("otmp", 3, F32); junk_b = mk("junk", 4, F32)

                        def psv(bk):
                            return bk[:] if DT_B == F32 else bk[:].bitcast(BF16)

                        def b1_ci(g, ci, qf_, kqf):
                            part = ci // 2
                            ct = part * 4 + hp2 * 2 + (ci % 2)
                            bk, bt = nb()
                            for c in range(8):
                                mm(bk[:], Wq[:, c, ci * 128:(ci + 1) * 128], XT[:, c, g * 512:(g + 1) * 512],
                                   [kWq, tXT[g]], [bt], start=(c == 0), stop=(c == 7))
                            H, kH = Hb.next()
                            yield
                            cp(H[:, 0:3], halo[:, ci, :], [khalo[ci]], [kH], eng="gpsimd")
                            act(H[:, 3:515], bk[:], AF.Copy, [bt], [kH])
                            cp(halo[:, ci, :], H[:, 512:515], [kH], [khalo[ci]], eng="gpsimd")
                            ac, kac = cacc.next()
                            yield
                            ts(ac[:], H[:, 0:512], cw[:, ct, 0:1], None, ALU.mult, None, [kH, kP], [kac])
                            for k in range(1, 4):
                                stt(ac[:], H[:, k:k + 512], cw[:, ct, k:k + 1], ac[:], ALU.mult, ALU.add, [kH, kP, kac], [kac])
                            if ci < 2:
                                dst = qf_[:, ci, :]; kd = kqf
                            else:
                                d_, kd = tmpS.next(); dst = d_[:]
                            yield
                            act(dst, ac[:], AF.Silu, [kac], [kd])
                            yield
                            bk2, bt2 = nb()
                            bv = psv(bk2)
                            for q in range(4):
                                tr(bv[:, q * 128:(q + 1) * 128], dst[:, q * 128:(q + 1) * 128], idn[:], [kd, tC], [bt2])
                            yield
                            cp(QKVt[:, :, ci * 128:(ci + 1) * 128], bv[:, 0:512].rearrange("p (q c) -> p q c", q=4),
                               [bt2], kQKV, eng=("vector" if ci % 2 else "scalar"))

                        def b1(g):
                            qf_, kqf = QfT.next()
                            run_il([b1_ci(g, ci, qf_, kqf) for ci in range(6)], B1W)
                            return qf_, kqf

                        FB, FG, FGC, FGS, FEG, FER, FEL, FSQ, FSK, FRQ0, FRK, FRKB, FRKBG, FRKD, FNB, FRQ, FRQE, FSSO, FRST = range(19)

                        def col(f, q, hl):
                            return f * 8 + q * 2 + hl

                        def gates_group(g):
                            s_, ks = gsm.next()
                            zns = []
                            bks = []
                            for q in range(4):
                                t = 4 * g + q
                                bk, bt = nb()
                                for c in range(8):
                                    mm(bk[:, 0:260], XT[:, c, t * 128:(t + 1) * 128], Wz[:, c, :], [tXT[g], kWz], [bt],
                                       start=(c == 0), stop=(c == 7))
                                zs_, kzs = zs.next(); zn_, kzn = zng.next()
                                act(zs_[:], bk[:, 0:256], AF.Silu, [bt], [kzs])
                                cp(s_[:, col(FB, q, 0):col(FB, q, 0) + 2], bk[:, 256:258], [bt], [ks])
                                tt(s_[:, col(FG, q, 0):col(FG, q, 0) + 2], bk[:, 258:260], dtb[:, 2 * hp2:2 * hp2 + 2], ALU.add,
                                   [bt, kP], [ks])
                                tt(zn_[:].rearrange("p (h d) -> p h d", h=2), zs_[:].rearrange("p (h d) -> p h d", h=2),
                                   ngt[:].unsqueeze(1).broadcast_to([128, 2, 128]), ALU.mult, [kzs, kP], [kzn])
                                zns.append((zn_, kzn))
                            for q in range(4):
                                jk, kjk = sqj.next()
                                act(jk[:], QKVt[:, q, 0:512], AF.Square, [kQKV[q]], [kjk])
                                S.op("vector", lambda e: e.tensor_reduce(out=s_[:, col(FSQ, q, 0):col(FSQ, q, 0) + 2],
                                                                         in_=jk[:, 0:256].rearrange("p (h d) -> p h d", h=2),
                                                                         axis=AX.X, op=ALU.add), [kjk, ks], [ks])
                                S.op("vector", lambda e: e.tensor_reduce(out=s_[:, col(FSK, q, 0):col(FSK, q, 0) + 2],
                                                                         in_=jk[:, 256:512].rearrange("p (h d) -> p h d", h=2),
                                                                         axis=AX.X, op=ALU.add), [kjk, ks], [ks])
                            f8 = lambda f: s_[:, f * 8:(f + 1) * 8]
                            act(f8(FB), f8(FB), AF.Exp, [ks], [ks], scale=-1.0)
                            ts(f8(FB), f8(FB), 1.0, None, ALU.add, None, [ks], [ks])
                            S.op("vector", lambda e: e.reciprocal(out=f8(FB), in_=f8(FB)), [ks], [ks])
                            act(f8(FG), f8(FG), AF.Exp, [ks], [ks])
                            act(f8(FG), f8(FG), AF.Ln, [ks], [ks], bias=1.0, scale=1.0)
                            tt(f8(FG).rearrange("p (q h) -> p q h", h=2), f8(FG).rearrange("p (q h) -> p q h", h=2),
                               nexpA[:, 2 * hp2:2 * hp2 + 2].unsqueeze(1).broadcast_to([128, 4, 2]), ALU.mult, [ks, kP], [ks])
                            bg, kbg_ = nb()
                            mm(bg[:, 0:8], trilT_f[:], f8(FG), [tC, ks], [kbg_])
                            mm(bg[:, 8:16], ones_f[:], f8(FG), [tC, ks], [kbg_])
                            cp(s_[:, FGC * 8:FGC * 8 + 16], bg[:, 0:16], [kbg_], [ks])
                            act(f8(FEG), f8(FGC), AF.Exp, [ks], [ks])
                            tt(f8(FER), f8(FGS), f8(FGC), ALU.subtract, [ks], [ks])
                            act(f8(FER), f8(FER), AF.Exp, [ks], [ks])
                            act(f8(FEL), f8(FGS), AF.Exp, [ks], [ks])
                            act(s_[:, FRQ0 * 8:FRQ0 * 8 + 16], s_[:, FSQ * 8:FSQ * 8 + 16], AF.Ln, [ks], [ks], bias=1e-6, scale=1.0)
                            act(s_[:, FRQ0 * 8:FRQ0 * 8 + 16], s_[:, FRQ0 * 8:FRQ0 * 8 + 16], AF.Exp, [ks], [ks], scale=-0.5)
                            tt(f8(FRKB), f8(FRK), f8(FB), ALU.mult, [ks], [ks])
                            tt(f8(FRKBG), f8(FRKB), f8(FEG), ALU.mult, [ks], [ks])
                            tt(f8(FRKD), f8(FRK), f8(FER), ALU.mult, [ks], [ks])
                            ts(f8(FNB), f8(FB), -1.0, None, ALU.mult, None, [ks], [ks])
                            ts(f8(FRQ), f8(FRQ0), 128.0 ** -0.5, None, ALU.mult, None, [ks], [ks])
                            tt(f8(FRQE), f8(FRQ), f8(FEG), ALU.mult, [ks], [ks])
                            return s_, ks, zns

                        def sc(s_, f, q, hl):
                            c = col(f, q, hl)
                            return s_[:, c:c + 1]

                        def prep_pre(t, hl, s_, ks, qf_, kqf, res):
                            q = t % 4
                            qk = QKVt[:, q, :]
                            kt_ = qk[:, 256 + hl * 128:256 + (hl + 1) * 128]
                            vt_ = qk[:, 512 + hl * 128:512 + (hl + 1) * 128]
                            kn_, kkn = kn_b.next(); kbg, kkbg = kbg_b.next(); kdec, kkdec = kdec_b.next()
                            vb, kvb = vb_b.next()
                            dg, kdg = dg_b.next()
                            act(kn_[:], kt_, AF.Copy, [kQKV[q], ks], [kkn], scale=sc(s_, FRK, q, hl))
                            ts(dg[:], ident_f[:], sc(s_, FGC, q, hl), None, ALU.mult, None, [tC, ks], [kdg], eng="gpsimd")
                            ts(kbg[:], kt_, sc(s_, FRKBG, q, hl), None, ALU.mult, None, [kQKV[q], ks], [kkbg])
                            act(kdec[:], kt_, AF.Copy, [kQKV[q], ks], [kkdec], scale=sc(s_, FRKD, q, hl))
                            ts(vb[:], vt_, sc(s_, FB, q, hl), None, ALU.mult, None, [kQKV[q], ks], [kvb])
                            yield
                            bB, kB = nb()
                            mm(bB[:, 0:128], ones_f[:], dg[:], [tC, kdg], [kB], start=True, stop=False)
                            mm(bB[:, 0:128], ident_f[:], mneg_f[:], [tC], [kB], start=False, stop=True)
                            yield
                            E, kE = E_b.next(); Es, kEs = Es_b.next()
                            act(E[:], bB[:, 0:128], AF.Exp, [kB, ks], [kE], scale=-1.0, bias=sc(s_, FGC, q, hl))
                            b1_, k1_ = nb()
                            b1v = psv(b1_)
                            tr(b1v[:, 0:128], kn_[:], idn[:], [kkn, tC], [k1_])
                            yield
                            knT, kknT = knT_b.next()
                            cp(knT[:], b1v[:, 0:128], [k1_], [kknT], eng="scalar")
                            tt(Es[:], E[:], sl_b[:], ALU.mult, [kE, tC], [kEs])
                            yield
                            bG, kG = nb()
                            mm(bG[:, 0:128], knT[:], knT[:], [kknT], [kG])
                            mm(bG[:, 128:256], qf_[:, hl, q * 128:(q + 1) * 128], knT[:], [kqf, kknT], [kG])
                            yield
                            A0, kA0 = A_b.next(); at_, kat = at_b.next()
                            stt(A0[:], bG[:, 0:128], sc(s_, FNB, q, hl), Es[:], ALU.mult, ALU.mult, [kG, ks, kEs], [kA0])
                            stt(at_[:], bG[:, 128:256], sc(s_, FRQ, q, hl), E[:], ALU.mult, ALU.mult, [kG, ks, kE], [kat])
                            yield
                            bT, kT = nb()
                            bTv = psv(bT)
                            tr(bTv[:, 0:128], A0[:], idn[:], [kA0, tC], [kT])
                            tr(bTv[:, 128:256], at_[:], idn[:], [kat, tC], [kT])
                            yield
                            B0, kB0 = A_b.next(); atT, katT = atT_b.next()
                            cp(B0[:], bTv[:, 0:128], [kT], [kB0], eng="scalar")
                            cp(atT[:], bTv[:, 128:256], [kT], [katT], eng="vector")
                            yield
                            P0, kP0 = P_b.next()
                            tt(P0[:], B0[:], idn[:], ALU.add, [kB0, tC], [kP0], eng="gpsimd")
                            res.update(dict(A=(A0, kA0), B=(B0, kB0), P=(P0, kP0), kbg=(kbg, kkbg), kdec=(kdec, kkdec),
                                            vb=(vb, kvb), atT=(atT, katT), hl=hl))

                        def solve_sq(sl, lev, flip):
                            (Ac, kAc), (Bc, kBc) = sl["A"], sl["B"]
                            bS, kSb = nb()
                            mm(bS[:, 0:128], Bc[:], Ac[:], [kBc, kAc], [kSb])
                            if lev < 5:
                                mm(bS[:, 128:256], Ac[:], Bc[:], [kBc, kAc], [kSb])
                            An, kAn = A_b.next()
                            e1 = "scalar" if flip else "vector"
                            cp(An[:], bS[:, 0:128], [kSb], [kAn], eng=e1)
                            if lev < 5:
                                Bn, kBn = A_b.next()
                                cp(Bn[:], bS[:, 128:256], [kSb], [kBn], eng=e1)
                            else:
                                Bn, kBn = None, None
                            sl["A"], sl["B"] = (An, kAn), (Bn, kBn)

                        def solve_pu(sl, lev):
                            (An, kAn), (Pc, kPc) = sl["A"], sl["P"]
                            bP, kPb = nb()
                            mm(bP[:, 0:128], An[:], Pc[:], [kAn, kPc], [kPb])
                            Pn, kPn = (P_b.next() if lev < 5 else TT_b.next())
                            tt(Pn[:], bP[:, 0:128], Pc[:], ALU.add, [kPb, kPc], [kPn])
                            sl["P"] = (Pn, kPn)

                        def post_group(slots):
                            for h0 in range(0, len(slots), 4):
                                part = slots[h0:h0 + 4]
                                bks = []
                                for sl in part:
                                    TT, kTT = sl["P"]
                                    kbg, kkbg = sl["kbg"]
                                    bw, kw = nb()
                                    mm(bw[:, 0:128], kbg[:], TT[:], [kkbg, kTT], [kw])
                                    bks.append((bw, kw))
                                for sl, (bw, kw) in zip(part, bks):
                                    nwT, knwT = nwT_b.next()
                                    act(nwT[:], bw[:, 0:128], AF.Copy, [kw], [knwT], scale=-1.0)
                                    sl["nwT"] = (nwT, knwT)

                        def scan_head(t, s_, ks, zn_, kzn, sl, qf_, kqf, yb_, kyb):
                            q = t % 4
                            hl = sl["hl"]
                            TT, kTT = sl["P"]; nwT, knwT = sl["nwT"]
                            kdec, kkdec = sl["kdec"]; vb, kvb = sl["vb"]; atT, katT = sl["atT"]
                            Sh = Sst[:, hl, :]
                            Shb = Sbf[:, hl, :]
                            bV, kVb = nb()
                            mm(bV[:, 0:128], TT[:], vb[:], [kTT, kvb], [kVb], start=True, stop=(t == 0))
                            if t > 0:
                                mm(bV[:, 0:128], nwT[:], Shb, [knwT, kS[hl]], [kVb], start=False, stop=True)
                            yield
                            vnew, kvn = vnew_b.next()
                            cp(vnew[:], bV[:, 0:128], [kVb], [kvn], eng="scalar")
                            yield
                            bO, kO = nb()
                            if t > 0:
                                mm(bO[:, 0:128], qf_[:, hl, q * 128:(q + 1) * 128], Shb, [kqf, kS[hl]], [kO])
                            mm(bO[:, 128:256], atT[:], vnew[:], [katT, kvn], [kO])
                            bS2, kS2 = nb()
                            mm(bS2[:, 0:128], kdec[:], vnew[:], [kkdec, kvn], [kS2])
                            yield
                            if t > 0:
                                if DT_B != F32:
                                    stt(Shb, Sh, sc(s_, FEL, q, hl), bS2[:, 0:128], ALU.mult, ALU.add, [kS[hl], ks, kS2], [kS[hl]])
                                stt(Sh, Sh, sc(s_, FEL, q, hl), bS2[:, 0:128], ALU.mult, ALU.add, [kS[hl], ks, kS2], [kS[hl]])
                            else:
                                if DT_B != F32:
                                    cp(Shb, bS2[:, 0:128], [kS2], [kS[hl]])
                                cp(Sh, bS2[:, 0:128], [kS2], [kS[hl]])
                            o_, ko_ = o_b.next()
                            if t > 0:
                                ot, kot = otmp_b.next()
                                ts(ot[:], bO[:, 0:128], sc(s_, FRQE, q, hl), None, ALU.mult, None, [kO, ks], [kot])
                                tt(o_[:], bO[:, 128:256], ot[:], ALU.add, [kO, kot], [ko_])
                            else:
                                cp(o_[:], bO[:, 128:256], [kO], [ko_])
                            yield
                            jk2, kjk2 = junk_b.next()
                            act(jk2[:], o_[:], AF.Square, [ko_], [kjk2, ks], accum_out=sc(s_, FSSO, q, hl))
                            act(sc(s_, FRST, q, hl), sc(s_, FSSO, q, hl), AF.Ln, [ks], [ks], bias=1e-6, scale=1.0 / 128.0)
                            act(sc(s_, FRST, q, hl), sc(s_, FRST, q, hl), AF.Exp, [ks], [ks], scale=-0.5)
                            yield
                            stt(yb_[:, hl * 128:(hl + 1) * 128], o_[:], sc(s_, FRST, q, hl), zn_[:, hl * 128:(hl + 1) * 128],
                                ALU.mult, ALU.mult, [ko_, ks, kzn], [kyb])

                        def scan_tile(info):
                            t, (s_, ks, zn_, kzn), slots, qf_, kqf = info
                            yb_, kyb = yb.next()
                            run_il([scan_head(t, s_, ks, zn_, kzn, sl, qf_, kqf, yb_, kyb) for sl in slots], 2)
                            yT_, kyT = ybT.next()
                            transpose_bf(yb_, kyb, 2, yT_, kyT)
                            outproj(t, yT_, kyT, WoB, kWo, 2, first_acc[t])
                            first_acc[t] = False

                        prev = None
                        for g in range(NG):
                            qf_, kqf = b1(g)
                            s_, ks, zns = gates_group(g)
                            tiles = []
                            gens = []
                            for q in range(4):
                                t = 4 * g + q
                                gi = (s_, ks, zns[q][0], zns[q][1])
                                sls = [dict(), dict()]
                                for hl in range(2):
                                    gens.append(prep_pre(t, hl, s_, ks, qf_, kqf, sls[hl]))
                                tiles.append((t, gi, sls, qf_, kqf))
                            run_il(gens, 4)
                            allslots = [s for ti in tiles for s in ti[2]]
                            for lev in range(6):
                                for si, s in enumerate(allslots):
                                    solve_sq(s, lev, si % 2)
                                for s in allslots:
                                    solve_pu(s, lev)
                                if prev is not None and lev < 4:
                                    scan_tile(prev[lev])
                            post_group(allslots)
                            prev = tiles
                        for q in range(4):
                            scan_tile(prev[q])
                        S.barrier()

            moe = (l % 2 == 1)
            li = l // 2
            if "F" in steps:
                with contextlib.ExitStack() as st:
                    def al(n, s, d):
                        return st.enter_context(nc.sbuf_tensor(f"F{l}_{n}", s, d))
                    g1 = al("g1", [128, D], F32); b1_ = al("b1", [128, D], F32)
                    g2 = al("g2", [128, D], F32); b2_ = al("b2", [128, D], F32); kLP = Tok()
                    S.dma("sync", g1[:], I["ln1_g"][l:l + 1, :].broadcast_to([128, D]), dw[2], writes=[kLP])
                    S.dma("sync", b1_[:], I["ln1_b"][l:l + 1, :].broadcast_to([128, D]), dw[2], writes=[kLP])
                    S.dma("sync", g2[:], I["ln2_g"][l:l + 1, :].broadcast_to([128, D]), dw[2], writes=[kLP])
                    S.dma("sync", b2_[:], I["ln2_b"][l:l + 1, :].broadcast_to([128, D]), dw[2], writes=[kLP])
                    kLP.w = (dw[2].name, dw[2].count)
                    sm = (al("st6", [128, NT, 2, 6], F32), al("mv", [128, NT, 2], F32), al("sd", [128, NT], F32),
                          al("rs", [128, NT], F32), [Tok() for _ in range(NT)])
                    comb = al("comb", [128, NT, NE], F32); kcomb = [Tok() for _ in range(NT)]
                    if moe:
                        Wr = al("Wr", [128, 8, NE], F32); kWr = Tok()
                        S.dma("sync", Wr[:], I["moe_router"][li].rearrange("(c p) e -> p c e", p=128), dw[2], writes=[kWr])
                        kWr.w = (dw[2].name, dw[2].count)
                        kLP.w = (dw[2].name, dw[2].count)
                        xtf = RB(al, "xtf", 2, [128, 512], F32)
                        lgb = {}
                        rsm = RB(al, "rsm", 2, [128, 64], F32)

                        def router(g, c, bk, bt):
                            if c == 0:
                                lgb["b"] = nb_special()
                            lb, klb = lgb["b"]
                            xf_, kxf = xtf.next()
                            ts(xf_[:], bk[:], 1.0 / ALPHA, None, ALU.mult, None, [bt], [kxf])
                            for q in range(4):
                                mm(lb[:, q * 8:(q + 1) * 8], xf_[:, q * 128:(q + 1) * 128], Wr[:, c, :], [kxf, kWr], [klb],
                                   start=(c == 0 and q == 0), stop=(c == 7), sgc=True)
                            if c == 7:
                                for q in range(4):
                                    t = 4 * g + q
                                    r_, kr_ = rsm.next()
                                    lg = r_[:, 0:8]
                                    cp(lg, lb[:, q * 8:(q + 1) * 8], [klb], [kr_])
                                    S.op("vector", lambda e: e.max(out=r_[:, 8:16], in_=lg), [kr_], [kr_])
                                    ts(r_[:, 16:24], lg, r_[:, 8:9], None, ALU.is_equal, None, [kr_], [kr_])
                                    ts(r_[:, 24:32], lg, r_[:, 9:10], None, ALU.is_equal, None, [kr_], [kr_])
                                    tt(r_[:, 32:33], r_[:, 9:10], r_[:, 8:9], ALU.subtract, [kr_], [kr_])
                                    act(r_[:, 32:33], r_[:, 32:33], AF.Exp, [kr_], [kr_])
                                    ts(r_[:, 33:34], r_[:, 32:33], 1.0, None, ALU.add, None, [kr_], [kr_])
                                    S.op("vector", lambda e: e.reciprocal(out=r_[:, 34:35], in_=r_[:, 33:34]), [kr_], [kr_])
                                    tt(r_[:, 35:36], r_[:, 32:33], r_[:, 34:35], ALU.mult, [kr_], [kr_])
                                    ts(r_[:, 16:24], r_[:, 16:24], r_[:, 34:35], None, ALU.mult, None, [kr_], [kr_])
                                    stt(comb[:, t, :], r_[:, 24:32], r_[:, 35:36], r_[:, 16:24], ALU.mult, ALU.add, [kr_], [kcomb[t]])
                    else:
                        router = None

                    ts(g1[:], g1[:], ALPHA, None, ALU.mult, None, [kLP], [kLP])
                    ts(b1_[:], b1_[:], ALPHA, None, ALU.mult, None, [kLP], [kLP])
                    groups = [(0, 4), (4, 4), (8, 4), (12, 4), (16, 4), (20, 2)]
                    Wg = RB(al, "Wg", 2, [128, 8, 512], BF16)
                    Wu = RB(al, "Wu", 2, [128, 8, 512], BF16)
                    Wd = RB(al, "Wd", 2, [128, 4, D], BF16)
                    actb = al("actb", [128, 4, SEQ], BF16); kact = [Tok() for _ in range(NG)]
                    sgb = RB(al, "sgb", 3, [128, 512], BF16)
                    experts = list(range(NE)) if moe else [None]
                    items = [(ex, c0, ncn) for ex in experts for (c0, ncn) in groups]
                    loaded = {}

                    def load_w(n):
                        if n >= len(items) or n in loaded:
                            return
                        ex, c0, ncn = items[n]
                        if moe:
                            wg_src = I["moe_w_gate"][li, ex]; wu_src = I["moe_w_up"][li, ex]; wd_src = I["moe_w_down"][li, ex]
                        else:
                            wg_src = I["ffn_w_gate"][li]; wu_src = I["ffn_w_up"][li]; wd_src = I["ffn_w_down"][li]
                        wgv = wg_src.rearrange("(c p) f -> p c f", p=128)
                        wuv = wu_src.rearrange("(c p) f -> p c f", p=128)
                        wdv = wd_src.rearrange("(c p) f -> p c f", p=128)
                        sl_i = n % 2
                        wg_, kwg = Wg.next(); wu_, kwu = Wu.next(); wd_, kwd = Wd.next()
                        S.dma("gpsimd", wg_[:, :, 0:ncn * 128], wgv[:, :, c0 * 128:(c0 + ncn) * 128], dw[sl_i], writes=[kwg])
                        S.dma("gpsimd", wu_[:, :, 0:ncn * 128], wuv[:, :, c0 * 128:(c0 + ncn) * 128], dw[sl_i], writes=[kwu])
                        S.dma("gpsimd", wd_[:, 0:ncn, :], wdv[:, c0:c0 + ncn, :], dw[sl_i], writes=[kwd])
                        fin = (dw[sl_i].name, dw[sl_i].count)
                        kwg.w = fin; kwu.w = fin; kwd.w = fin
                        loaded[n] = (wg_, kwg, wu_, kwu, wd_, kwd)

                    load_w(0)
                    load_w(1)

                    for g in range(NG):
                        run_il([ln_gen(4 * g + q, g1, b1_, kLP, sm) for q in range(4)], 4)
                        make_xt(g, router, scale=1.0 / ALPHA)

                    for n, (ex, c0, ncn) in enumerate(items):
                        if True:
                            if True:
                                wg_, kwg, wu_, kwu, wd_, kwd = loaded[n]
                            for g in range(NG):
                                for cc in range(ncn):
                                    pg, kpg = nb()
                                    for c in range(8):
                                        mm(pg[:], wg_[:, c, cc * 128:(cc + 1) * 128], XT[:, c, g * 512:(g + 1) * 512],
                                           [kwg, tXT[g]], [kpg], start=(c == 0), stop=(c == 7))
                                    pu, kpu = nb()
                                    for c in range(8):
                                        mm(pu[:], wu_[:, c, cc * 128:(cc + 1) * 128], XT[:, c, g * 512:(g + 1) * 512],
                                           [kwu, tXT[g]], [kpu], start=(c == 0), stop=(c == 7))
                                    sg_, ksg = sgb.next()
                                    act(sg_[:], pg[:], AF.Silu, [kpg], [ksg])
                                    tt(actb[:, cc, g * 512:(g + 1) * 512], sg_[:], pu[:], ALU.mult, [ksg, kpu], [kact[g]])
                            for t in range(NT):
                                g = t // 4
                                for hf in range(2):
                                    bk, bt = nb()
                                    for cc in range(ncn):
                                        mm(bk[:], actb[:, cc, t * 128:(t + 1) * 128], wd_[:, cc, hf * 512:(hf + 1) * 512],
                                           [kact[g], kwd], [bt], start=(cc == 0), stop=(cc == ncn - 1))
                                    xs = X[:, t, hf * 512:(hf + 1) * 512]
                                    if moe:
                                        stt(xs, bk[:], comb[:, t, ex:ex + 1], xs, ALU.mult, ALU.add, [bt, kcomb[t], tX[t]], [tX[t]])
                                    else:
                                        tt(xs, bk[:], xs, ALU.add, [bt, tX[t]], [tX[t]])
                        load_w(n + 2)
                    last = (l == n_layers - 1)
                    outv = out.rearrange("(t p) d -> p t d", p=128)
                    def store_tile(t):
                        S.dma("sync", outv[:, t, :], X[:, t, :], dout, reads=[tX[t]])
                    def ln2_group(g):
                        run_il([ln_gen(4 * g + q, g2, b2_, kLP, sm, after=(store_tile if last else None))
                                for q in range(4)], 4)
                    for g in range(NG):
                        ln2_group(g)
                        if not last:
                            make_xt(g)
                    S.barrier(rotate=not last)
            elif dbg:
                pass

        if "F" not in steps:
            outv = out.rearrange("(t p) d -> p t d", p=128)
            for t in range(NT):
                S.dma("sync", outv[:, t, :], X[:, t, :], dout, reads=[tX[t]])
        S.wait_events("sync", [(dout.name, dout.count)])
        S.barrier()
        print("instr counts", S.ninst, "gen", S.gen)
    return nc


def prep_inputs(inputs, n_layers=DEPTH):
    f = lambda a: np.ascontiguousarray(np.asarray(a, dtype=np.float32))
    shared = {}
    for k in ("w_in", "a_ln_g", "a_ln_b", "b_a_log", "b_dt_bias", "b_norm_g", "w_out", "ln1_g", "ln1_b", "ln2_g",
              "ln2_b", "ffn_w_gate", "ffn_w_up", "ffn_w_down", "moe_router", "moe_w_gate", "moe_w_up", "moe_w_down"):
        shared[k] = f(inputs[k])
    cw = np.asarray(inputs["conv_w"], np.float32)
    shared["conv_w"] = f(cw.reshape(DEPTH, 4, 12, 128).transpose(0, 3, 2, 1))
    ws = np.asarray(inputs["a_ws"], np.float32)
    shared["a_ws"] = f(ws.transpose(0, 3, 1, 2))
    shared["a_bs"] = f(np.asarray(inputs["a_bs"], np.float32).transpose(0, 2, 1))
    if n_layers < DEPTH:
        for k in list(shared.keys()):
            shp = small_shape(k, IN_SHAPES[k], n_layers)
            sl = tuple(slice(0, n) for n in shp)
            shared[k] = f(shared[k][sl])
    shared.update(host_consts())
    x = np.asarray(inputs["x"], np.float32)
    in_maps = []
    for b in range(8):
        m = dict(shared)
        m["x"] = f(x[b])
        in_maps.append(m)
    return in_maps


def kernel(**inputs):
    in_maps = prep_inputs(inputs)
    nc = build()
    res = run_bass_kernel_spmd(nc, in_maps, core_ids=list(range(8)))
    return np.stack([np.asarray(r["out"], dtype=np.float32) for r in res.results], axis=0)
```

```python
import contextlib
import math
import numpy as np
import concourse.bass as bass
import concourse.mybir as mybir
from concourse.bass_utils import run_bass_kernel_spmd

F32 = mybir.dt.float32
BF16 = mybir.dt.bfloat16
AF = mybir.ActivationFunctionType
ALU = mybir.AluOpType
AX = mybir.AxisListType

D = 1024
SEQ = 2048
NT = 16
NG = 4
DEPTH = 4
D_IN = 3336
D_FF = 2816
NFC = 22
NE = 8
ALPHA = (2.0 * DEPTH) ** 0.25
DT_B = BF16


class Tok:
    __slots__ = ("name", "w", "r", "excl")

    def __init__(self, name="", excl=False):
        self.name = name
        self.w = None
        self.r = []
        self.excl = excl


class DSem:
    def __init__(self, name, handle):
        self.name = name
        self.handle = handle
        self.count = 0


class Sched:
    ENG = ("tensor", "vector", "scalar", "gpsimd", "sync")

    def __init__(self, nc, stack):
        self.nc = nc
        self.stack = stack
        self.gen = 0
        self.handles = {}
        self.retired = set()
        self.dsems = []
        self.ninst = {e: 0 for e in self.ENG}
        self._new_sems()

    def _new_sems(self):
        self.key = {}
        self.cnt = {}
        for e in self.ENG:
            k = f"{e}#{self.gen}"
            self.key[e] = k
            self.handles[k] = self.stack.enter_context(self.nc.semaphore(f"s_{e}_{self.gen}"))
            self.cnt[e] = 0
        self.waited = {e: {} for e in self.ENG}

    def dsem(self, name):
        h = self.stack.enter_context(self.nc.semaphore("d_" + name))
        ds = DSem("d_" + name, h)
        self.handles[ds.name] = h
        self.dsems.append(ds)
        return ds

    def _waits(self, e, reads, writes):
        need = {}
        mykey = self.key[e]

        def req(ev, same_ok):
            key, val = ev
            if key in self.retired:
                return
            if key == mykey and same_ok and e == "tensor":
                return
            if need.get(key, 0) < val:
                need[key] = val

        for t in reads:
            if t.w is not None:
                req(t.w, False)
            if t.excl:
                for ev in t.r:
                    if ev[0] != mykey:
                        req(ev, False)
        for t in writes:
            if t.w is not None:
                req(t.w, True)
            for ev in t.r:
                req(ev, True)
        out = []
        w = self.waited[e]
        for key, val in need.items():
            if w.get(key, 0) < val:
                w[key] = val
                out.append((self.handles[key], val))
        return out

    def _emit(self, e, waits, fn, sem, inc):
        eng = getattr(self.nc, e)
        for h, val in waits:
            eng.wait_ge(h, val)
            self.ninst[e] += 1
        if fn is not None:
            fn(eng).then_inc(sem, inc)
            self.ninst[e] += 1

    def op(self, e, fn, reads=(), writes=()):
        waits = self._waits(e, reads, writes)
        self.cnt[e] += 1
        ev = (self.key[e], self.cnt[e])
        self._emit(e, waits, fn, self.handles[self.key[e]], 1)
        for t in reads:
            t.r.append(ev)
            if len(t.r) > 32:
                mx = {}
                for k_, v_ in t.r:
                    if k_ not in self.retired and mx.get(k_, 0) < v_:
                        mx[k_] = v_
                t.r = list(mx.items())
        for t in writes:
            t.w = ev
            t.r = []
        return ev

    def dma(self, q, out, in_, ds, reads=(), writes=(), **kw):
        waits = self._waits(q, reads, writes)
        ds.count += 16
        ev = (ds.name, ds.count)
        self._emit(q, waits, lambda eng: eng.dma_start(out=out, in_=in_, **kw), ds.handle, 16)
        for t in reads:
            t.r.append(ev)
        for t in writes:
            t.w = ev
            t.r = []
        return ev

    def wait_events(self, e, events):
        tok = Tok("w")
        for ev in events:
            tok.w = ev
            waits = self._waits(e, [tok], [])
            self._emit(e, waits, None, None, 0)

    def barrier(self, rotate=False):
        evs = [(self.key[e], self.cnt[e]) for e in self.ENG if self.cnt[e] > 0]
        evs += [(ds.name, ds.count) for ds in self.dsems if ds.count > 0]
        for e in self.ENG:
            self.wait_events(e, evs)
        if rotate:
            for e in self.ENG:
                self.retired.add(self.key[e])
            self.gen += 1
            self._new_sems()
            for e in self.ENG:
                for ds in self.dsems:
                    self.waited[e][ds.name] = ds.count


def run_il(gens, width):
    it = iter(gens)
    active = []
    while True:
        while len(active) < width:
            try:
                active.append(next(it))
            except StopIteration:
                break
        if not active:
            break
        nxt = []
        for gn in active:
            try:
                next(gn)
                nxt.append(gn)
            except StopIteration:
                pass
        active = nxt


class RB:
    def __init__(self, alloc, name, n, shape, dt):
        self.t = [alloc(f"{name}{i}", shape, dt) for i in range(n)]
        self.k = [Tok(f"{name}{i}") for i in range(n)]
        self.i = 0

    def next(self):
        i = self.i
        self.i = (i + 1) % len(self.t)
        return self.t[i], self.k[i]


def host_consts():
    c = {}
    i = np.arange(128)
    c["ident"] = np.eye(128, dtype=np.float32)
    c["trilT"] = (i[:, None] <= i[None, :]).astype(np.float32)
    c["ones"] = np.ones((128, 128), np.float32)
    c["mneg"] = np.where(i[None, :] <= i[:, None], 0.0, 30000.0).astype(np.float32)
    c["sl"] = (i[None, :] < i[:, None]).astype(np.float32)
    inv = 10000.0 ** (-np.arange(0, 64, 2, dtype=np.float32) / 64.0)
    ang = np.arange(SEQ, dtype=np.float32)[:, None] * inv[None, :]
    c["cos"] = np.ascontiguousarray(np.cos(ang).astype(np.float32).reshape(16, 128, 32).transpose(1, 0, 2))
    c["sin"] = np.ascontiguousarray(np.sin(ang).astype(np.float32).reshape(16, 128, 32).transpose(1, 0, 2))
    u = np.arange(SEQ)[None, :] - i[:, None]
    m = ((u >= 0) & (u <= 128)).astype(np.float32)
    m += ((u >= 0) & (u <= 512) & (u % 4 == 0)).astype(np.float32)
    m += ((u >= 0) & (u <= 2048) & (u % 16 == 0)).astype(np.float32)
    c["mstrip"] = m.astype(np.float32)
    return c


CONST_SHAPES = {"ident": [128, 128], "trilT": [128, 128], "ones": [128, 128], "mneg": [128, 128],
                "sl": [128, 128], "cos": [128, 16, 32], "sin": [128, 16, 32], "mstrip": [128, SEQ]}

IN_SHAPES = {
    "x": [SEQ, D], "w_in": [DEPTH, D, D_IN], "conv_w": [DEPTH, 128, 12, 4], "a_ln_g": [DEPTH, 256],
    "a_ln_b": [DEPTH, 256], "a_ws": [DEPTH, 128, 4, 128], "a_bs": [DEPTH, 128, 4], "b_a_log": [DEPTH, 4],
    "b_dt_bias": [DEPTH, 4], "b_norm_g": [DEPTH, 128], "w_out": [DEPTH, D, D], "ln1_g": [DEPTH, D],
    "ln1_b": [DEPTH, D], "ln2_g": [DEPTH, D], "ln2_b": [DEPTH, D], "ffn_w_gate": [2, D, D_FF],
    "ffn_w_up": [2, D, D_FF], "ffn_w_down": [2, D_FF, D], "moe_router": [2, D, NE],
    "moe_w_gate": [2, NE, D, D_FF], "moe_w_up": [2, NE, D, D_FF], "moe_w_down": [2, NE, D_FF, D],
}


import os
KDBG = os.environ.get("KDBG", "")


def small_shape(k, shp, n_layers):
    if n_layers >= DEPTH or k == "x":
        return list(shp)
    shp = list(shp)
    if k.startswith("ffn_"):
        shp[0] = (n_layers + 1) // 2
        if "nof" in KDBG:
            shp = [1, 8, 8]
    elif k.startswith("moe_"):
        shp[0] = max(1, n_layers // 2)
        if n_layers < 2:
            shp = [1] + [1] * (len(shp) - 3) + shp[-2:] if len(shp) == 4 else shp
    else:
        shp[0] = n_layers
    return shp


def build(n_layers=DEPTH, steps="ACBF", dbg=False):
    nc = bass.Bass("TRN2", target_bir_lowering=False)
    I = {}
    for k, shp in list(IN_SHAPES.items()):
        I[k] = nc.dram_tensor(k, small_shape(k, shp, n_layers), F32, kind="ExternalInput").ap()
    for k, shp in list(CONST_SHAPES.items()):
        I[k] = nc.dram_tensor(k, shp, F32, kind="ExternalInput").ap()
    out = nc.dram_tensor("out", [SEQ, D], F32, kind="ExternalOutput").ap()

    with contextlib.ExitStack() as gst:
        S = Sched(nc, gst)

        def galloc(n, s, d):
            return gst.enter_context(nc.sbuf_tensor(n, s, d))

        X = galloc("X", [128, NT, D], F32)
        XT = galloc("XT", [128, 8, SEQ], BF16)
        tX = [Tok(f"X{t}") for t in range(NT)]
        tXT = [Tok(f"XT{g}") for g in range(NG)]
        ident_f = galloc("ident_f", [128, 128], F32)
        ident_b = galloc("ident_b", [128, 128], BF16)
        trilT_f = galloc("trilT_f", [128, 128], F32)
        ones_f = galloc("ones_f", [128, 128], F32)
        mneg_f = galloc("mneg_f", [128, 128], F32)
        sl_b = galloc("sl_b", [128, 128], DT_B)
        tC = Tok("consts")
        banks = [gst.enter_context(nc.psum_tensor(f"bank{i}", [128, 512], F32)) for i in range(8)]
        bank_tok = [Tok(f"bank{i}", excl=True) for i in range(8)]
        bstate = {"i": 0}

        def nb():
            i = bstate["i"]
            bstate["i"] = (i + 1) % 6
            return banks[i], bank_tok[i]

        def nb_special():
            i = 6 + bstate.get("s", 0)
            bstate["s"] = bstate.get("s", 0) ^ 1
            return banks[i], bank_tok[i]

        def mm(o, lhsT, rhs, r, w, start=True, stop=True, sgc=False):
            S.op("tensor", lambda e: e.matmul(o, lhsT=lhsT, rhs=rhs, start=start, stop=stop, skip_group_check=sgc), r, w)

        def tr(o, i_, idn, r, w):
            S.op("tensor", lambda e: e.transpose(out=o, in_=i_, identity=idn), r, w)

        def act(o, i_, func, r, w, **kw):
            S.op("scalar", lambda e: e.activation(out=o, in_=i_, func=func, **kw), r, w)

        def ts(o, i0, s1, s2, op0, op1, r, w, eng="vector"):
            if op1 is None:
                S.op(eng, lambda e: e.tensor_scalar(out=o, in0=i0, scalar1=s1, scalar2=None, op0=op0), r, w)
            else:
                S.op(eng, lambda e: e.tensor_scalar(out=o, in0=i0, scalar1=s1, scalar2=s2, op0=op0, op1=op1), r, w)

        def tt(o, i0, i1, op, r, w, eng="vector"):
            S.op(eng, lambda e: e.tensor_tensor(out=o, in0=i0, in1=i1, op=op), r, w)

        def stt(o, i0, sc, i1, op0, op1, r, w):
            S.op("vector", lambda e: e.scalar_tensor_tensor(out=o, in0=i0, scalar=sc, in1=i1, op0=op0, op1=op1), r, w)

        def cp(o, i_, r, w, eng="vector"):
            if eng == "scalar":
                S.op("scalar", lambda e: e.copy(out=o, in_=i_), r, w)
            else:
                S.op(eng, lambda e: e.tensor_copy(out=o, in_=i_), r, w)

        dconst = S.dsem("const")
        dx = S.dsem("x")
        dw = [S.dsem(f"w{i}") for i in range(6)]
        dout = S.dsem("out")

        with contextlib.ExitStack() as st0:
            sl_f = st0.enter_context(nc.sbuf_tensor("sl_f", [128, 128], F32))
            tmpk = Tok()
            S.dma("sync", ident_f[:], I["ident"], dconst, writes=[tC])
            S.dma("sync", trilT_f[:], I["trilT"], dconst, writes=[tC])
            S.dma("sync", ones_f[:], I["ones"], dconst, writes=[tC])
            S.dma("sync", mneg_f[:], I["mneg"], dconst, writes=[tC])
            S.dma("sync", sl_f[:], I["sl"], dconst, writes=[tmpk])
            tC.w = (dconst.name, dconst.count)
            tmpk.w = (dconst.name, dconst.count)
            cp(ident_b[:], ident_f[:], [tC], [tC])
            cp(sl_b[:], sl_f[:], [tmpk], [tC])
            xv = I["x"].rearrange("(t p) d -> p t d", p=128)
            for q in range(4):
                S.dma("sync" if q % 2 == 0 else "scalar", X[:, 4 * q:4 * q + 4, :], xv[:, 4 * q:4 * q + 4, :], dx,
                      writes=tX[4 * q:4 * q + 4])
            for t in range(NT):
                tX[t].w = (dx.name, dx.count)
            S.barrier()

        def make_xt(g, router=None, scale=None):
            for c in range(8):
                bk, bt = nb()
                for q in range(4):
                    t = 4 * g + q
                    tr(bk[:, q * 128:(q + 1) * 128], X[:, t, c * 128:(c + 1) * 128], ident_f[:], [tX[t], tC], [bt])
                dst = XT[:, c, g * 512:(g + 1) * 512]
                if scale is None:
                    cp(dst, bk[:], [bt], [tXT[g]], eng=("scalar" if c % 2 == 0 else "vector"))
                elif c % 2 == 0:
                    act(dst, bk[:], AF.Copy, [bt], [tXT[g]], scale=scale)
                else:
                    ts(dst, bk[:], scale, None, ALU.mult, None, [bt], [tXT[g]])
                if router is not None:
                    router(g, c, bk, bt)

        def ln_gen(t, gam, bet, tpar, sm, eps=1e-5, after=None):
            st6, mv, sd, rs, ksm = sm
            for hf in range(2):
                S.op("vector", lambda e: e.bn_stats(out=st6[:, t, hf, :], in_=X[:, t, hf * 512:(hf + 1) * 512]),
                     [tX[t]], [ksm[t]])
            S.op("vector", lambda e: e.bn_aggr(out=mv[:, t, :], in_=st6[:, t, :, :]), [ksm[t]], [ksm[t]])
            yield
            act(sd[:, t:t + 1], mv[:, t, 1:2], AF.Sqrt, [ksm[t]], [ksm[t]], bias=eps, scale=1.0)
            yield
            S.op("vector", lambda e: e.reciprocal(out=rs[:, t:t + 1], in_=sd[:, t:t + 1]), [ksm[t]], [ksm[t]])
            ts(sd[:, t:t + 1], mv[:, t, 0:1], -1.0, rs[:, t:t + 1], ALU.mult, ALU.mult, [ksm[t]], [ksm[t]])
            yield
            act(X[:, t, :], X[:, t, :], AF.Identity, [tX[t], ksm[t]], [tX[t]], scale=rs[:, t:t + 1], bias=sd[:, t:t + 1])
            yield
            tt(X[:, t, :], X[:, t, :], gam[:], ALU.mult, [tX[t], tpar], [tX[t]])
            yield
            tt(X[:, t, :], X[:, t, :], bet[:], ALU.add, [tX[t], tpar], [tX[t]], eng="gpsimd")
            if after is not None:
                yield
                after(t)

        def accum_X(t, hf, bk, bt, first, cscal=None):
            xs = X[:, t, hf * 512:(hf + 1) * 512]
            if first:
                stt(xs, xs, ALPHA, bk[:], ALU.mult, ALU.add, [tX[t], bt], [tX[t]])
            elif cscal is not None:
                stt(xs, bk[:], cscal, xs, ALU.mult, ALU.add, [tX[t], bt], [tX[t]])
            else:
                tt(xs, bk[:], xs, ALU.add, [tX[t], bt], [tX[t]])

        def outproj(t, yT, kyT, Wo, kWo, nk, first):
            for hf in range(2):
                bk, bt = nb()
                for k in range(nk):
                    mm(bk[:], yT[:, k, :], Wo[:, k, hf * 512:(hf + 1) * 512], [kyT, kWo], [bt],
                       start=(k == 0), stop=(k == nk - 1))
                accum_X(t, hf, bk, bt, first)

        def transpose_bf(src, ksrc, nk, dst, kdst, eng="scalar"):
            bk, bt = nb()
            bkb = bk[:].bitcast(BF16)
            for k in range(nk):
                tr(bkb[:, k * 128:(k + 1) * 128], src[:, k * 128:(k + 1) * 128], ident_b[:], [ksrc, tC], [bt])
            cp(dst[:], bkb[:, 0:nk * 128].rearrange("p (k c) -> p k c", k=nk), [bt], [kdst], eng=eng)

        for g in range(NG):
            make_xt(g)

        for l in range(n_layers):
            winv = I["w_in"][l].rearrange("(c p) f -> p c f", p=128)
            woutv = I["w_out"][l].rearrange("(c p) f -> p c f", p=128)
            first_acc = [True] * NT

            if "A" in steps:
                with contextlib.ExitStack() as st:
                    def al(n, s, d):
                        return st.enter_context(nc.sbuf_tensor(f"A{l}_{n}", s, d))
                    WA = al("WA", [128, 8, 512], BF16); kWA = Tok()
                    WoA = al("WoA", [128, 2, D], BF16); kWo = Tok()
                    Wsf = al("Wsf", [128, 4, 128], F32)
                    WsT = al("WsT", [128, 4, 128], BF16); kWs = Tok()
                    bsA = al("bsA", [128, 4], F32)
                    lng = al("lng", [128, 256], F32)
                    lnb = al("lnb", [128, 256], F32); kP = Tok()
                    S.dma("gpsimd", WA[:], winv[:, :, 0:512], dw[0], writes=[kWA])
                    S.dma("gpsimd", WoA[:], woutv[:, 0:2, :], dw[1], writes=[kWo])
                    S.dma("sync", Wsf[:], I["a_ws"][l], dw[2], writes=[kWs])
                    S.dma("sync", bsA[:], I["a_bs"][l], dw[2], writes=[kP])
                    S.dma("sync", lng[:], I["a_ln_g"][l:l + 1, :].broadcast_to([128, 256]), dw[2], writes=[kP])
                    S.dma("sync", lnb[:], I["a_ln_b"][l:l + 1, :].broadcast_to([128, 256]), dw[2], writes=[kP])
                    kWs.w = (dw[2].name, dw[2].count)
                    kP.w = (dw[2].name, dw[2].count)
                    tt(WsT[:], Wsf[:], trilT_f[:].unsqueeze(1).broadcast_to([128, 4, 128]), ALU.mult, [kWs, tC], [kWs])
                    WIL = 4
                    sq = RB(al, "sq", WIL, [128, 512], F32)
                    inn = RB(al, "inn", WIL, [128, 512], F32)
                    ge = RB(al, "ge", WIL, [128, 512], F32)
                    vn = RB(al, "vn", WIL, [128, 256], BF16)
                    ya = RB(al, "ya", WIL, [128, 256], BF16)
                    yaT = RB(al, "yaT", WIL, [128, 2, 128], BF16)
                    sm = RB(al, "smA", WIL, [128, 16], F32)

                    def a_tile(t):
                        g = t // 4
                        p1, k1 = nb()
                        for c in range(8):
                            mm(p1[:], XT[:, c, t * 128:(t + 1) * 128], WA[:, c, :], [tXT[g], kWA], [k1],
                               start=(c == 0), stop=(c == 7))
                        sq_, ksq = sq.next(); in_, kin = inn.next(); ge_, kge = ge.next()
                        yield
                        act(sq_[:], p1[:], AF.Square, [k1], [ksq])
                        yield
                        ts(in_[:], sq_[:], 0.044715, 1.0, ALU.mult, ALU.add, [ksq], [kin])
                        tt(in_[:], in_[:], p1[:], ALU.mult, [kin, k1], [kin])
                        yield
                        act(sq_[:], in_[:], AF.Sigmoid, [kin], [ksq], scale=1.5957691216057308)
                        yield
                        tt(ge_[:], sq_[:], p1[:], ALU.mult, [ksq, k1], [kge])
                        s_, ks = sm.next()
                        S.op("vector", lambda e: e.bn_stats(out=s_[:, 0:6], in_=ge_[:, 256:512]), [kge], [ks])
                        S.op("vector", lambda e: e.bn_aggr(out=s_[:, 6:8], in_=s_[:, 0:6]), [ks], [ks])
                        yield
                        act(s_[:, 8:9], s_[:, 7:8], AF.Sqrt, [ks], [ks], bias=1e-5, scale=1.0)
                        yield
                        S.op("vector", lambda e: e.reciprocal(out=s_[:, 9:10], in_=s_[:, 8:9]), [ks], [ks])
                        ts(in_[:, 0:256], ge_[:, 256:512], s_[:, 6:7], s_[:, 9:10], ALU.subtract, ALU.mult,
                           [kge, ks], [kin])
                        yield
                        tt(in_[:, 0:256], in_[:, 0:256], lng[:], ALU.mult, [kin, kP], [kin], eng="gpsimd")
                        vn_, kvn = vn.next()
                        tt(vn_[:], in_[:, 0:256], lnb[:], ALU.add, [kin, kP], [kvn], eng="gpsimd")
                        yield
                        p2, k2 = nb()
                        for gi in range(4):
                            mm(p2[:, gi * 64:(gi + 1) * 64], WsT[:, gi, :], vn_[:, gi * 64:(gi + 1) * 64], [kWs, kvn], [k2])
                        yield
                        ya_, kya = ya.next()
                        for gi in range(4):
                            stt(ya_[:, gi * 64:(gi + 1) * 64], p2[:, gi * 64:(gi + 1) * 64], bsA[:, gi:gi + 1],
                                ge_[:, gi * 64:(gi + 1) * 64], ALU.add, ALU.mult, [k2, kP, kge], [kya])
                        yield
                        yT_, kyT = yaT.next()
                        transpose_bf(ya_, kya, 2, yT_, kyT)
                        yield
                        outproj(t, yT_, kyT, WoA, kWo, 2, first_acc[t])
                        first_acc[t] = False

                    run_il([a_tile(t) for t in range(NT)], WIL)
                    S.barrier()

            if "C" in steps:
                with contextlib.ExitStack() as st:
                    def al(n, s, d):
                        return st.enter_context(nc.sbuf_tensor(f"C{l}_{n}", s, d))
                    WC = al("WC", [128, 8, 768], BF16); kWC = Tok()
                    WoC = al("WoC", [128, 2, D], BF16); kWo = Tok()
                    QT = al("QT", [128, 2, SEQ], BF16); kQT = [Tok() for _ in range(NT)]
                    KT = al("KT", [128, 2, SEQ], BF16); kKT = [Tok() for _ in range(NT)]
                    Vp = al("Vp", [128, NT, 4, 65], BF16); kV = [Tok() for _ in range(NT)]
                    Mst = al("Mst", [128, SEQ], BF16); kM = Tok()
                    cosT = al("cosT", [128, NT, 32], F32)
                    sinT = al("sinT", [128, NT, 32], F32); kcs = Tok()
                    S.dma("gpsimd", WC[:], winv[:, :, 2568:3336], dw[0], writes=[kWC])
                    S.dma("gpsimd", WoC[:], woutv[:, 6:8, :], dw[1], writes=[kWo])
                    S.dma("gpsimd", Mst[:, 0:1024], I["mstrip"][:, 0:1024], dw[3], writes=[kM])
                    S.dma("gpsimd", Mst[:, 1024:2048], I["mstrip"][:, 1024:2048], dw[3], writes=[kM])
                    kM.w = (dw[3].name, dw[3].count)
                    S.dma("sync", cosT[:], I["cos"], dw[2], writes=[kcs])
                    S.dma("sync", sinT[:], I["sin"], dw[2], writes=[kcs])
                    kcs.w = (dw[2].name, dw[2].count)
                    kVall = Tok()
                    S.op("gpsimd", lambda e: e.memset(Vp[:], 1.0), [], [kVall])
                    for t in range(NT):
                        kV[t].w = kVall.w
                    qf = RB(al, "qf", 4, [128, 256], F32)
                    kf = RB(al, "kf", 4, [128, 256], F32)
                    r1 = RB(al, "r1", 8, [128, 4, 32], F32)
                    r2 = RB(al, "r2", 8, [128, 4, 32], F32)
                    r3 = RB(al, "r3", 8, [128, 4, 32], F32)
                    r4 = RB(al, "r4", 8, [128, 4, 32], F32)
                    qr = RB(al, "qr", 4, [128, 256], BF16)
                    kr = RB(al, "kr", 4, [128, 256], BF16)
                    Pb = RB(al, "Pb", 6, [128, 512], BF16)
                    YC = RB(al, "YC", 2, [128, 4, 256], BF16)
                    ycT = RB(al, "ycT", 2, [128, 2, 128], BF16)
                    rc = RB(al, "rc", 4, [128, 4], F32)

                    def rope(src, ksrc, dst, kdst, t):
                        x4 = src[:].rearrange("p (h a i) -> p h a i", h=4, a=2)
                        o4 = dst[:].rearrange("p (h a i) -> p h a i", h=4, a=2)
                        x1 = x4[:, :, 0, :]; x2 = x4[:, :, 1, :]
                        cb = cosT[:, t:t + 1, :].broadcast_to([128, 4, 32])
                        sb_ = sinT[:, t:t + 1, :].broadcast_to([128, 4, 32])
                        a_, ka = r1.next(); b_, kb = r2.next(); c_, kc = r3.next(); d_, kd = r4.next()
                        tt(a_[:], x1, cb, ALU.mult, [ksrc, kcs], [ka])
                        tt(b_[:], x2, sb_, ALU.mult, [ksrc, kcs], [kb])
                        tt(c_[:], x2, cb, ALU.mult, [ksrc, kcs], [kc], eng="gpsimd")
                        tt(d_[:], x1, sb_, ALU.mult, [ksrc, kcs], [kd], eng="gpsimd")
                        tt(o4[:, :, 0, :], a_[:], b_[:], ALU.subtract, [ka, kb], [kdst])
                        tt(o4[:, :, 1, :], c_[:], d_[:], ALU.add, [kc, kd], [kdst])

                    def c_proj(t):
                        g = t // 4
                        pq, kpq = nb()
                        for c in range(8):
                            mm(pq[:, 0:256], XT[:, c, t * 128:(t + 1) * 128], WC[:, c, 0:256], [tXT[g], kWC], [kpq],
                               start=(c == 0), stop=(c == 7))
                        pkv, kpkv = nb()
                        for c in range(8):
                            mm(pkv[:], XT[:, c, t * 128:(t + 1) * 128], WC[:, c, 256:768], [tXT[g], kWC], [kpkv],
                               start=(c == 0), stop=(c == 7))
                        qf_, kqf = qf.next(); kf_, kkf = kf.next()
                        yield
                        act(qf_[:], pq[:, 0:256], AF.Copy, [kpq], [kqf], scale=0.125)
                        act(kf_[:], pkv[:, 0:256], AF.Copy, [kpkv], [kkf])
                        cp(Vp[:, t, :, 0:64], pkv[:, 256:512].rearrange("p (h d) -> p h d", h=4), [kpkv], [kV[t]])
                        qr_, kqr = qr.next(); kr_, kkr = kr.next()
                        yield
                        rope(qf_, kqf, qr_, kqr, t)
                        yield
                        rope(kf_, kkf, kr_, kkr, t)
                        yield
                        bk, bt = nb()
                        bkb = bk[:].bitcast(BF16)
                        for k in range(2):
                            tr(bkb[:, k * 128:(k + 1) * 128], qr_[:, k * 128:(k + 1) * 128], ident_b[:], [kqr, tC], [bt])
                        for k in range(2):
                            tr(bkb[:, 256 + k * 128:256 + (k + 1) * 128], kr_[:, k * 128:(k + 1) * 128], ident_b[:],
                               [kkr, tC], [bt])
                        yield
                        cp(QT[:, :, t * 128:(t + 1) * 128], bkb[:, 0:256].rearrange("p (k c) -> p k c", k=2), [bt],
                           [kQT[t]], eng="scalar")
                        cp(KT[:, :, t * 128:(t + 1) * 128], bkb[:, 256:512].rearrange("p (k c) -> p k c", k=2), [bt],
                           [kKT[t]], eng="vector")

                    mask_flip = [0]

                    def attn_head(g, h, yc_, kyc):
                        hp = h // 2; hb = 64 * (h % 2)
                        ob, ko = nb_special()
                        ov = ob[:, 0:260].rearrange("p (i e) -> p i e", e=65)
                        nj = 4 * g + 4

                        def score(j):
                            i0 = max(j, 4 * g)
                            n = (4 * g + 4 - i0) * 128
                            sc, ksc = nb()
                            mm(sc[:, 0:n], KT[hb:hb + 64, hp, j * 128:(j + 1) * 128],
                               QT[hb:hb + 64, hp, i0 * 128:(4 * g + 4) * 128],
                               [kKT[j]] + kQT[i0:4 * g + 4], [ksc])
                            return sc, ksc, i0, n

                        firstmm = True
                        pend = score(0)
                        for j in range(nj):
                            sc, ksc, i0, n = pend
                            p_, kp = Pb.next()
                            act(p_[:, 0:n], sc[:, 0:n], AF.Exp, [ksc], [kp])
                            if j + 1 < nj:
                                pend = score(j + 1)
                            yield
                            u0 = 128 * (i0 - j)
                            mask_flip[0] = (mask_flip[0] + 1) % 3
                            tt(p_[:, 0:n], p_[:, 0:n], Mst[:, u0:u0 + n], ALU.mult, [kp, kM], [kp],
                               eng=("gpsimd" if mask_flip[0] == 0 else "vector"))
                            yield
                            for i in range(i0, 4 * g + 4):
                                mm(ov[:, i - 4 * g, :], p_[:, (i - i0) * 128:(i - i0 + 1) * 128], Vp[:, j, h, :],
                                   [kp, kV[j]], [ko], start=firstmm, stop=(j == i), sgc=True)
                                firstmm = False
                            yield
                        rc_, krc = rc.next()
                        S.op("vector", lambda e: e.reciprocal(out=rc_[:], in_=ov[:, :, 64]), [ko], [krc])
                        for i in range(4):
                            ts(yc_[:, i, h * 64:(h + 1) * 64], ov[:, i, 0:64], rc_[:, i:i + 1], None, ALU.mult, None,
                               [ko, krc], [kyc])

                    def attn_group(g):
                        yc_, kyc = YC.next()
                        if "Cnoh1" in KDBG:
                            run_il([attn_head(g, h, yc_, kyc) for h in (0, 2)], 2)
                        else:
                            run_il([attn_head(g, h, yc_, kyc) for h in range(4)], 2)
                        for i in range(4):
                            t = 4 * g + i
                            yT_, kyT = ycT.next()
                            transpose_bf(yc_[:, i, :], kyc, 2, yT_, kyT)
                            outproj(t, yT_, kyT, WoC, kWo, 2, first_acc[t])
                            first_acc[t] = False

                    for g in range(NG):
                        run_il([c_proj(4 * g + q) for q in range(4)], 2)
                        if "Cproj" not in KDBG:
                            attn_group(g)
                    S.barrier()

            if "B" in steps:
                for hp2 in range(2):
                    with contextlib.ExitStack() as st:
                        def al(n, s, d):
                            return st.enter_context(nc.sbuf_tensor(f"B{l}{hp2}_{n}", s, d))
                        idn = ident_f if DT_B == F32 else ident_b
                        Wq = al("Wq", [128, 8, 768], BF16); kWq = Tok()
                        Wz = al("Wz", [128, 8, 260], BF16); kWz = Tok()
                        WoB = al("WoB", [128, 2, D], BF16); kWo = Tok()
                        cw = al("cw", [128, 12, 4], F32)
                        ngt = al("ngt", [128, 128], F32)
                        alog = al("alog", [128, 4], F32)
                        dtb = al("dtb", [128, 4], F32)
                        nexpA = al("nexpA", [128, 4], F32); kP = Tok()
                        for pi, base in enumerate((512, 1024, 1536)):
                            S.dma("gpsimd", Wq[:, :, pi * 256:(pi + 1) * 256],
                                  winv[:, :, base + hp2 * 256: base + (hp2 + 1) * 256], dw[0], writes=[kWq])
                        kWq.w = (dw[0].name, dw[0].count)
                        S.dma("gpsimd", Wz[:, :, 0:256], winv[:, :, 2048 + hp2 * 256:2048 + (hp2 + 1) * 256], dw[1], writes=[kWz])
                        S.dma("gpsimd", Wz[:, :, 256:258], winv[:, :, 2560 + 2 * hp2:2562 + 2 * hp2], dw[1], writes=[kWz])
                        S.dma("gpsimd", Wz[:, :, 258:260], winv[:, :, 2564 + 2 * hp2:2566 + 2 * hp2], dw[1], writes=[kWz])
                        kWz.w = (dw[1].name, dw[1].count)
                        S.dma("gpsimd", WoB[:], woutv[:, 2 + 2 * hp2:4 + 2 * hp2, :], dw[3], writes=[kWo])
                        S.dma("sync", cw[:], I["conv_w"][l], dw[2], writes=[kP])
                        S.dma("sync", ngt[:], I["b_norm_g"][l:l + 1, :].broadcast_to([128, 128]), dw[2], writes=[kP])
                        S.dma("sync", alog[:], I["b_a_log"][l:l + 1, :].broadcast_to([128, 4]), dw[2], writes=[kP])
                        S.dma("sync", dtb[:], I["b_dt_bias"][l:l + 1, :].broadcast_to([128, 4]), dw[2], writes=[kP])
                        kP.w = (dw[2].name, dw[2].count)
                        act(nexpA[:], alog[:], AF.Exp, [kP], [kP])
                        ts(nexpA[:], nexpA[:], -1.0, None, ALU.mult, None, [kP], [kP])

                        NCH = 4
                        NS = 2 * NCH
                        halo = al("halo", [128, 6, 3], F32); khalo = [Tok() for _ in range(6)]
                        S.op("gpsimd", lambda e: e.memset(halo[:], 0.0), [], khalo)
                        B1W = 2
                        Hb = RB(al, "Hb", B1W, [128, 515], F32)
                        cacc = RB(al, "cacc", B1W, [128, 512], F32)
                        QfT = RB(al, "QfT", 2, [128, 2, 512], DT_B)
                        tmpS = RB(al, "tmpS", B1W, [128, 512], DT_B)
                        QKVt = al("QKVt", [128, 4, 768], DT_B); kQKV = [Tok() for _ in range(4)]
                        Sst = al("Sst", [128, 2, 128], F32); kS = [Tok(), Tok()]
                        if DT_B != F32:
                            Sbf = al("Sbf", [128, 2, 128], DT_B)
                        else:
                            Sbf = Sst
                        zng = RB(al, "zng", 2 * NCH, [128, 256], BF16)
                        zs = RB(al, "zs", 2, [128, 256], F32)
                        gsm = RB(al, "gsm", 3, [128, 160], F32)
                        sqj = RB(al, "sqj", 2, [128, 512], BF16)
                        yb = RB(al, "yb", 2, [128, 256], BF16)
                        ybT = RB(al, "ybT", 2, [128, 2, 128], BF16)

                        def mk(nm, n, dt=DT_B):
                            return RB(al, nm, n, [128, 128], dt)
                        kn_b = mk("kn", NS); knT_b = mk("knT", NS); E_b = mk("E", NS); Es_b = mk("Es", NS)
                        at_b = mk("attn", NS); kbg_b = mk("kbg", NS); dg_b = mk("dg", 8, F32)
                        A_b = mk("Ab", 4 * NS); P_b = mk("Pb", 2 * NS); TT_b = mk("TTb", 2 * NS)
                        kdec_b = mk("kdec", 2 * NS); vb_b = mk("vb", 2 * NS); atT_b = mk("attnT", 2 * NS); nwT_b = mk("nwT", 2 * NS)
                        vnew_b = mk("vnew", 4); o_b = mk("o", 3, F32); otmp_b = mk("otmp", 3, F32); junk_b = mk("junk", 4, F32)

                        def psv(bk):
                            return bk[:] if DT_B == F32 else bk[:].bitcast(BF16)

                        def b1_ci(g, ci, qf_, kqf):
                            part = ci // 2
                            ct = part * 4 + hp2 * 2 + (ci % 2)
                            bk, bt = nb()
                            for c in range(8):
                                mm(bk[:], Wq[:, c, ci * 128:(ci + 1) * 128], XT[:, c, g * 512:(g + 1) * 512],
                                   [kWq, tXT[g]], [bt], start=(c == 0), stop=(c == 7))
                            H, kH = Hb.next()
                            yield
                            cp(H[:, 0:3], halo[:, ci, :], [khalo[ci]], [kH], eng="gpsimd")
                            act(H[:, 3:515], bk[:], AF.Copy, [bt], [kH])
                            cp(halo[:, ci, :], H[:, 512:515], [kH], [khalo[ci]], eng="gpsimd")
                            ac, kac = cacc.next()
                            yield
                            ts(ac[:], H[:, 0:512], cw[:, ct, 0:1], None, ALU.mult, None, [kH, kP], [kac])
                            for k in range(1, 4):
                                stt(ac[:], H[:, k:k + 512], cw[:, ct, k:k + 1], ac[:], ALU.mult, ALU.add, [kH, kP, kac], [kac])
                            if ci < 2:
                                dst = qf_[:, ci, :]; kd = kqf
                            else:
                                d_, kd = tmpS.next(); dst = d_[:]
                            yield
                            act(dst, ac[:], AF.Silu, [kac], [kd])
                            yield
                            bk2, bt2 = nb()
                            bv = psv(bk2)
                            for q in range(4):
                                tr(bv[:, q * 128:(q + 1) * 128], dst[:, q * 128:(q + 1) * 128], idn[:], [kd, tC], [bt2])
                            yield
                            cp(QKVt[:, :, ci * 128:(ci + 1) * 128], bv[:, 0:512].rearrange("p (q c) -> p q c", q=4),
                               [bt2], kQKV, eng=("vector" if ci % 2 else "scalar"))

                        def b1(g):
                            qf_, kqf = QfT.next()
                            run_il([b1_ci(g, ci, qf_, kqf) for ci in range(6)], B1W)
                            return qf_, kqf

                        FB, FG, FGC, FGS, FEG, FER, FEL, FSQ, FSK, FRQ0, FRK, FRKB, FRKBG, FRKD, FNB, FRQ, FRQE, FSSO, FRST = range(19)

                        def col(f, q, hl):
                            return f * 8 + q * 2 + hl

                        def gates_group(g):
                            s_, ks = gsm.next()
                            zns = []
                            bks = []
                            for q in range(4):
                                t = 4 * g + q
                                bk, bt = nb()
                                for c in range(8):
                                    mm(bk[:, 0:260], XT[:, c, t * 128:(t + 1) * 128], Wz[:, c, :], [tXT[g], kWz], [bt],
                                       start=(c == 0), stop=(c == 7))
                                zs_, kzs = zs.next(); zn_, kzn = zng.next()
                                act(zs_[:], bk[:, 0:256], AF.Silu, [bt], [kzs])
                                cp(s_[:, col(FB, q, 0):col(FB, q, 0) + 2], bk[:, 256:258], [bt], [ks])
                                tt(s_[:, col(FG, q, 0):col(FG, q, 0) + 2], bk[:, 258:260], dtb[:, 2 * hp2:2 * hp2 + 2], ALU.add,
                                   [bt, kP], [ks])
                                tt(zn_[:].rearrange("p (h d) -> p h d", h=2), zs_[:].rearrange("p (h d) -> p h d", h=2),
                                   ngt[:].unsqueeze(1).broadcast_to([128, 2, 128]), ALU.mult, [kzs, kP], [kzn])
                                zns.append((zn_, kzn))
                            for q in range(4):
                                jk, kjk = sqj.next()
                                act(jk[:], QKVt[:, q, 0:512], AF.Square, [kQKV[q]], [kjk])
                                S.op("vector", lambda e: e.tensor_reduce(out=s_[:, col(FSQ, q, 0):col(FSQ, q, 0) + 2],
                                                                         in_=jk[:, 0:256].rearrange("p (h d) -> p h d", h=2),
                                                                         axis=AX.X, op=ALU.add), [kjk, ks], [ks])
                                S.op("vector", lambda e: e.tensor_reduce(out=s_[:, col(FSK, q, 0):col(FSK, q, 0) + 2],
                                                                         in_=jk[:, 256:512].rearrange("p (h d) -> p h d", h=2),
                                                                         axis=AX.X, op=ALU.add), [kjk, ks], [ks])
                            f8 = lambda f: s_[:, f * 8:(f + 1) * 8]
                            act(f8(FB), f8(FB), AF.Exp, [ks], [ks], scale=-1.0)
                            ts(f8(FB), f8(FB), 1.0, None, ALU.add, None, [ks], [ks])
                            S.op("vector", lambda e: e.reciprocal(out=f8(FB), in_=f8(FB)), [ks], [ks])
                            act(f8(FG), f8(FG), AF.Exp, [ks], [ks])
                            act(f8(FG), f8(FG), AF.Ln, [ks], [ks], bias=1.0, scale=1.0)
                            tt(f8(FG).rearrange("p (q h) -> p q h", h=2), f8(FG).rearrange("p (q h) -> p q h", h=2),
                               nexpA[:, 2 * hp2:2 * hp2 + 2].unsqueeze(1).broadcast_to([128, 4, 2]), ALU.mult, [ks, kP], [ks])
                            bg, kbg_ = nb()
                            mm(bg[:, 0:8], trilT_f[:], f8(FG), [tC, ks], [kbg_])
                            mm(bg[:, 8:16], ones_f[:], f8(FG), [tC, ks], [kbg_])
                            cp(s_[:, FGC * 8:FGC * 8 + 16], bg[:, 0:16], [kbg_], [ks])
                            act(f8(FEG), f8(FGC), AF.Exp, [ks], [ks])
                            tt(f8(FER), f8(FGS), f8(FGC), ALU.subtract, [ks], [ks])
                            act(f8(FER), f8(FER), AF.Exp, [ks], [ks])
                            act(f8(FEL), f8(FGS), AF.Exp, [ks], [ks])
                            act(s_[:, FRQ0 * 8:FRQ0 * 8 + 16], s_[:, FSQ * 8:FSQ * 8 + 16], AF.Ln, [ks], [ks], bias=1e-6, scale=1.0)
                            act(s_[:, FRQ0 * 8:FRQ0 * 8 + 16], s_[:, FRQ0 * 8:FRQ0 * 8 + 16], AF.Exp, [ks], [ks], scale=-0.5)
                            tt(f8(FRKB), f8(FRK), f8(FB), ALU.mult, [ks], [ks])
                            tt(f8(FRKBG), f8(FRKB), f8(FEG), ALU.mult, [ks], [ks])
                            tt(f8(FRKD), f8(FRK), f8(FER), ALU.mult, [ks], [ks])
                            ts(f8(FNB), f8(FB), -1.0, None, ALU.mult, None, [ks], [ks])
                            ts(f8(FRQ), f8(FRQ0), 128.0 ** -0.5, None, ALU.mult, None, [ks], [ks])
                            tt(f8(FRQE), f8(FRQ), f8(FEG), ALU.mult, [ks], [ks])
                            return s_, ks, zns

                        def sc(s_, f, q, hl):
                            c = col(f, q, hl)
                            return s_[:, c:c + 1]

                        def prep_pre(t, hl, s_, ks, qf_, kqf, res):
                            q = t % 4
                            qk = QKVt[:, q, :]
                            kt_ = qk[:, 256 + hl * 128:256 + (hl + 1) * 128]
                            vt_ = qk[:, 512 + hl * 128:512 + (hl + 1) * 128]
                            kn_, kkn = kn_b.next(); kbg, kkbg = kbg_b.next(); kdec, kkdec = kdec_b.next()
                            vb, kvb = vb_b.next()
                            dg, kdg = dg_b.next()
                            act(kn_[:], kt_, AF.Copy, [kQKV[q], ks], [kkn], scale=sc(s_, FRK, q, hl))
                            ts(dg[:], ident_f[:], sc(s_, FGC, q, hl), None, ALU.mult, None, [tC, ks], [kdg], eng="gpsimd")
                            ts(kbg[:], kt_, sc(s_, FRKBG, q, hl), None, ALU.mult, None, [kQKV[q], ks], [kkbg])
                            act(kdec[:], kt_, AF.Copy, [kQKV[q], ks], [kkdec], scale=sc(s_, FRKD, q, hl))
                            ts(vb[:], vt_, sc(s_, FB, q, hl), None, ALU.mult, None, [kQKV[q], ks], [kvb])
                            yield
                            bB, kB = nb()
                            mm(bB[:, 0:128], ones_f[:], dg[:], [tC, kdg], [kB], start=True, stop=False)
                            mm(bB[:, 0:128], ident_f[:], mneg_f[:], [tC], [kB], start=False, stop=True)
                            yield
                            E, kE = E_b.next(); Es, kEs = Es_b.next()
                            act(E[:], bB[:, 0:128], AF.Exp, [kB, ks], [kE], scale=-1.0, bias=sc(s_, FGC, q, hl))
                            b1_, k1_ = nb()
                            b1v = psv(b1_)
                            tr(b1v[:, 0:128], kn_[:], idn[:], [kkn, tC], [k1_])
                            yield
                            knT, kknT = knT_b.next()
                            cp(knT[:], b1v[:, 0:128], [k1_], [kknT], eng="scalar")
                            tt(Es[:], E[:], sl_b[:], ALU.mult, [kE, tC], [kEs])
                            yield
                            bG, kG = nb()
                            mm(bG[:, 0:128], knT[:], knT[:], [kknT], [kG])
                            mm(bG[:, 128:256], qf_[:, hl, q * 128:(q + 1) * 128], knT[:], [kqf, kknT], [kG])
                            yield
                            A0, kA0 = A_b.next(); at_, kat = at_b.next()
                            stt(A0[:], bG[:, 0:128], sc(s_, FNB, q, hl), Es[:], ALU.mult, ALU.mult, [kG, ks, kEs], [kA0])
                            stt(at_[:], bG[:, 128:256], sc(s_, FRQ, q, hl), E[:], ALU.mult, ALU.mult, [kG, ks, kE], [kat])
                            yield
                            bT, kT = nb()
                            bTv = psv(bT)
                            tr(bTv[:, 0:128], A0[:], idn[:], [kA0, tC], [kT])
                            tr(bTv[:, 128:256], at_[:], idn[:], [kat, tC], [kT])
                            yield
                            B0, kB0 = A_b.next(); atT, katT = atT_b.next()
                            cp(B0[:], bTv[:, 0:128], [kT], [kB0], eng="scalar")
                            cp(atT[:], bTv[:, 128:256], [kT], [katT], eng="vector")
                            yield
                            P0, kP0 = P_b.next()
                            tt(P0[:], B0[:], idn[:], ALU.add, [kB0, tC], [kP0], eng="gpsimd")
                            res.update(dict(A=(A0, kA0), B=(B0, kB0), P=(P0, kP0), kbg=(kbg, kkbg), kdec=(kdec, kkdec),
                                            vb=(vb, kvb), atT=(atT, katT), hl=hl))

                        def solve_sq(sl, lev, flip):
                            (Ac, kAc), (Bc, kBc) = sl["A"], sl["B"]
                            bS, kSb = nb()
                            mm(bS[:, 0:128], Bc[:], Ac[:], [kBc, kAc], [kSb])
                            if lev < 5:
                                mm(bS[:, 128:256], Ac[:], Bc[:], [kBc, kAc], [kSb])
                            An, kAn = A_b.next()
                            e1 = "scalar" if flip else "vector"
                            cp(An[:], bS[:, 0:128], [kSb], [kAn], eng=e1)
                            if lev < 5:
                                Bn, kBn = A_b.next()
                                cp(Bn[:], bS[:, 128:256], [kSb], [kBn], eng=e1)
                            else:
                                Bn, kBn = None, None
                            sl["A"], sl["B"] = (An, kAn), (Bn, kBn)

                        def solve_pu(sl, lev):
                            (An, kAn), (Pc, kPc) = sl["A"], sl["P"]
                            bP, kPb = nb()
                            mm(bP[:, 0:128], An[:], Pc[:], [kAn, kPc], [kPb])
                            Pn, kPn = (P_b.next() if lev < 5 else TT_b.next())
                            tt(Pn[:], bP[:, 0:128], Pc[:], ALU.add, [kPb, kPc], [kPn])
                            sl["P"] = (Pn, kPn)

                        def post_group(slots):
                            for h0 in range(0, len(slots), 4):
                                part = slots[h0:h0 + 4]
                                bks = []
                                for sl in part:
                                    TT, kTT = sl["P"]
                                    kbg, kkbg = sl["kbg"]
                                    bw, kw = nb()
                                    mm(bw[:, 0:128], kbg[:], TT[:], [kkbg, kTT], [kw])
                                    bks.append((bw, kw))
                                for sl, (bw, kw) in zip(part, bks):
                                    nwT, knwT = nwT_b.next()
                                    act(nwT[:], bw[:, 0:128], AF.Copy, [kw], [knwT], scale=-1.0)
                                    sl["nwT"] = (nwT, knwT)

                        def scan_head(t, s_, ks, zn_, kzn, sl, qf_, kqf, yb_, kyb):
                            q = t % 4
                            hl = sl["hl"]
                            TT, kTT = sl["P"]; nwT, knwT = sl["nwT"]
                            kdec, kkdec = sl["kdec"]; vb, kvb = sl["vb"]; atT, katT = sl["atT"]
                            Sh = Sst[:, hl, :]
                            Shb = Sbf[:, hl, :]
                            bV, kVb = nb()
                            mm(bV[:, 0:128], TT[:], vb[:], [kTT, kvb], [kVb], start=True, stop=(t == 0))
                            if t > 0:
                                mm(bV[:, 0:128], nwT[:], Shb, [knwT, kS[hl]], [kVb], start=False, stop=True)
                            yield
                            vnew, kvn = vnew_b.next()
                            cp(vnew[:], bV[:, 0:128], [kVb], [kvn], eng="scalar")
                            yield
                            bO, kO = nb()
                            if t > 0:
                                mm(bO[:, 0:128], qf_[:, hl, q * 128:(q + 1) * 128], Shb, [kqf, kS[hl]], [kO])
                            mm(bO[:, 128:256], atT[:], vnew[:], [katT, kvn], [kO])
                            bS2, kS2 = nb()
                            mm(bS2[:, 0:128], kdec[:], vnew[:], [kkdec, kvn], [kS2])
                            yield
                            if t > 0:
                                if DT_B != F32:
                                    stt(Shb, Sh, sc(s_, FEL, q, hl), bS2[:, 0:128], ALU.mult, ALU.add, [kS[hl], ks, kS2], [kS[hl]])
                                stt(Sh, Sh, sc(s_, FEL, q, hl), bS2[:, 0:128], ALU.mult, ALU.add, [kS[hl], ks, kS2], [kS[hl]])
                            else:
                                if DT_B != F32:
                                    cp(Shb, bS2[:, 0:128], [kS2], [kS[hl]])
                                cp(Sh, bS2[:, 0:128], [kS2], [kS[hl]])
                            o_, ko_ = o_b.next()
                            if t > 0:
                                ot, kot = otmp_b.next()
                                ts(ot[:], bO[:, 0:128], sc(s_, FRQE, q, hl), None, ALU.mult, None, [kO, ks], [kot])
                                tt(o_[:], bO[:, 128:256], ot[:], ALU.add, [kO, kot], [ko_])
                            else:
                                cp(o_[:], bO[:, 128:256], [kO], [ko_])
                            yield
                            jk2, kjk2 = junk_b.next()
                            act(jk2[:], o_[:], AF.Square, [ko_], [kjk2, ks], accum_out=sc(s_, FSSO, q, hl))
                            act(sc(s_, FRST, q, hl), sc(s_, FSSO, q, hl), AF.Ln, [ks], [ks], bias=1e-6, scale=1.0 / 128.0)
                            act(sc(s_, FRST, q, hl), sc(s_, FRST, q, hl), AF.Exp, [ks], [ks], scale=-0.5)
                            yield
                            stt(yb_[:, hl * 128:(hl + 1) * 128], o_[:], sc(s_, FRST, q, hl), zn_[:, hl * 128:(hl + 1) * 128],
                                ALU.mult, ALU.mult, [ko_, ks, kzn], [kyb])

                        def scan_tile(info):
                            t, (s_, ks, zn_, kzn), slots, qf_, kqf = info
                            yb_, kyb = yb.next()
                            run_il([scan_head(t, s_, ks, zn_, kzn, sl, qf_, kqf, yb_, kyb) for sl in slots], 2)
                            yT_, kyT = ybT.next()
                            transpose_bf(yb_, kyb, 2, yT_, kyT)
                            outproj(t, yT_, kyT, WoB, kWo, 2, first_acc[t])
                            first_acc[t] = False

                        prev = None
                        for g in range(NG):
                            qf_, kqf = b1(g)
                            s_, ks, zns = gates_group(g)
                            tiles = []
                            gens = []
                            for q in range(4):
                                t = 4 * g + q
                                gi = (s_, ks, zns[q][0], zns[q][1])
                                sls = [dict(), dict()]
                                for hl in range(2):
                                    gens.append(prep_pre(t, hl, s_, ks, qf_, kqf, sls[hl]))
                                tiles.append((t, gi, sls, qf_, kqf))
                            run_il(gens, 4)
                            allslots = [s for ti in tiles for s in ti[2]]
                            for lev in range(6):
                                for si, s in enumerate(allslots):
                                    solve_sq(s, lev, si % 2)
                                for s in allslots:
                                    solve_pu(s, lev)
                                if prev is not None and lev < 4:
                                    scan_tile(prev[lev])
                            post_group(allslots)
                            prev = tiles
                        for q in range(4):
                            scan_tile(prev[q])
                        S.barrier()

            moe = (l % 2 == 1)
            li = l // 2
            if "F" in steps:
                with contextlib.ExitStack() as st:
                    def al(n, s, d):
                        return st.enter_context(nc.sbuf_tensor(f"F{l}_{n}", s, d))
                    g1 = al("g1", [128, D], F32); b1_ = al("b1", [128, D], F32)
                    g2 = al("g2", [128, D], F32); b2_ = al("b2", [128, D], F32); kLP = Tok()
                    S.dma("sync", g1[:], I["ln1_g"][l:l + 1, :].broadcast_to([128, D]), dw[2], writes=[kLP])
                    S.dma("sync", b1_[:], I["ln1_b"][l:l + 1, :].broadcast_to([128, D]), dw[2], writes=[kLP])
                    S.dma("sync", g2[:], I["ln2_g"][l:l + 1, :].broadcast_to([128, D]), dw[2], writes=[kLP])
                    S.dma("sync", b2_[:], I["ln2_b"][l:l + 1, :].broadcast_to([128, D]), dw[2], writes=[kLP])
                    kLP.w = (dw[2].name, dw[2].count)
                    sm = (al("st6", [128, NT, 2, 6], F32), al("mv", [128, NT, 2], F32), al("sd", [128, NT], F32),
                          al("rs", [128, NT], F32), [Tok() for _ in range(NT)])
                    comb = al("comb", [128, NT, NE], F32); kcomb = [Tok() for _ in range(NT)]
                    if moe:
                        Wr = al("Wr", [128, 8, NE], F32); kWr = Tok()
                        S.dma("sync", Wr[:], I["moe_router"][li].rearrange("(c p) e -> p c e", p=128), dw[2], writes=[kWr])
                        kWr.w = (dw[2].name, dw[2].count)
                        kLP.w = (dw[2].name, dw[2].count)
                        xtf = RB(al, "xtf", 2, [128, 512], F32)
                        lgb = {}
                        rsm = RB(al, "rsm", 2, [128, 64], F32)

                        def router(g, c, bk, bt):
                            if c == 0:
                                lgb["b"] = nb_special()
                            lb, klb = lgb["b"]
                            xf_, kxf = xtf.next()
                            ts(xf_[:], bk[:], 1.0 / ALPHA, None, ALU.mult, None, [bt], [kxf])
                            for q in range(4):
                                mm(lb[:, q * 8:(q + 1) * 8], xf_[:, q * 128:(q + 1) * 128], Wr[:, c, :], [kxf, kWr], [klb],
                                   start=(c == 0 and q == 0), stop=(c == 7), sgc=True)
                            if c == 7:
                                for q in range(4):
                                    t = 4 * g + q
                                    r_, kr_ = rsm.next()
                                    lg = r_[:, 0:8]
                                    cp(lg, lb[:, q * 8:(q + 1) * 8], [klb], [kr_])
                                    S.op("vector", lambda e: e.max(out=r_[:, 8:16], in_=lg), [kr_], [kr_])
                                    ts(r_[:, 16:24], lg, r_[:, 8:9], None, ALU.is_equal, None, [kr_], [kr_])
                                    ts(r_[:, 24:32], lg, r_[:, 9:10], None, ALU.is_equal, None, [kr_], [kr_])
                                    tt(r_[:, 32:33], r_[:, 9:10], r_[:, 8:9], ALU.subtract, [kr_], [kr_])
                                    act(r_[:, 32:33], r_[:, 32:33], AF.Exp, [kr_], [kr_])
                                    ts(r_[:, 33:34], r_[:, 32:33], 1.0, None, ALU.add, None, [kr_], [kr_])
                                    S.op("vector", lambda e: e.reciprocal(out=r_[:, 34:35], in_=r_[:, 33:34]), [kr_], [kr_])
                                    tt(r_[:, 35:36], r_[:, 32:33], r_[:, 34:35], ALU.mult, [kr_], [kr_])
                                    ts(r_[:, 16:24], r_[:, 16:24], r_[:, 34:35], None, ALU.mult, None, [kr_], [kr_])
                                    stt(comb[:, t, :], r_[:, 24:32], r_[:, 35:36], r_[:, 16:24], ALU.mult, ALU.add, [kr_], [kcomb[t]])
                    else:
                        router = None

                    ts(g1[:], g1[:], ALPHA, None, ALU.mult, None, [kLP], [kLP])
                    ts(b1_[:], b1_[:], ALPHA, None, ALU.mult, None, [kLP], [kLP])
                    groups = [(0, 4), (4, 4), (8, 4), (12, 4), (16, 4), (20, 2)]
                    Wg = RB(al, "Wg", 2, [128, 8, 512], BF16)
                    Wu = RB(al, "Wu", 2, [128, 8, 512], BF16)
                    Wd = RB(al, "Wd", 2, [128, 4, D], BF16)
                    actb = al("actb", [128, 4, SEQ], BF16); kact = [Tok() for _ in range(NG)]
                    sgb = RB(al, "sgb", 3, [128, 512], BF16)
                    experts = list(range(NE)) if moe else [None]
                    items = [(ex, c0, ncn) for ex in experts for (c0, ncn) in groups]
                    loaded = {}

                    def load_w(n):
                        if n >= len(items) or n in loaded:
                            return
                        ex, c0, ncn = items[n]
                        if moe:
                            wg_src = I["moe_w_gate"][li, ex]; wu_src = I["moe_w_up"][li, ex]; wd_src = I["moe_w_down"][li, ex]
                        else:
                            wg_src = I["ffn_w_gate"][li]; wu_src = I["ffn_w_up"][li]; wd_src = I["ffn_w_down"][li]
                        wgv = wg_src.rearrange("(c p) f -> p c f", p=128)
                        wuv = wu_src.rearrange("(c p) f -> p c f", p=128)
                        wdv = wd_src.rearrange("(c p) f -> p c f", p=128)
                        sl_i = n % 2
                        wg_, kwg = Wg.next(); wu_, kwu = Wu.next(); wd_, kwd = Wd.next()
                        S.dma("gpsimd", wg_[:, :, 0:ncn * 128], wgv[:, :, c0 * 128:(c0 + ncn) * 128], dw[sl_i], writes=[kwg])
                        S.dma("gpsimd", wu_[:, :, 0:ncn * 128], wuv[:, :, c0 * 128:(c0 + ncn) * 128], dw[sl_i], writes=[kwu])
                        S.dma("gpsimd", wd_[:, 0:ncn, :], wdv[:, c0:c0 + ncn, :], dw[sl_i], writes=[kwd])
                        fin = (dw[sl_i].name, dw[sl_i].count)
                        kwg.w = fin; kwu.w = fin; kwd.w = fin
                        loaded[n] = (wg_, kwg, wu_, kwu, wd_, kwd)

                    load_w(0)
                    load_w(1)

                    for g in range(NG):
                        run_il([ln_gen(4 * g + q, g1, b1_, kLP, sm) for q in range(4)], 4)
                        make_xt(g, router, scale=1.0 / ALPHA)

                    def up_phase(n, g):
                        ex, c0, ncn = items[n]
                        wg_, kwg, wu_, kwu, wd_, kwd = loaded[n]
                        for cc in range(ncn):
                            pg, kpg = nb()
                            for c in range(8):
                                mm(pg[:], wg_[:, c, cc * 128:(cc + 1) * 128], XT[:, c, g * 512:(g + 1) * 512],
                                   [kwg, tXT[g]], [kpg], start=(c == 0), stop=(c == 7))
                            pu, kpu = nb()
                            for c in range(8):
                                mm(pu[:], wu_[:, c, cc * 128:(cc + 1) * 128], XT[:, c, g * 512:(g + 1) * 512],
                                   [kwu, tXT[g]], [kpu], start=(c == 0), stop=(c == 7))
                            sg_, ksg = sgb.next()
                            act(sg_[:], pg[:], AF.Silu, [kpg], [ksg])
                            tt(actb[:, cc, g * 512:(g + 1) * 512], sg_[:], pu[:], ALU.mult, [ksg, kpu], [kact[g]])

                    def down_phase(n, g):
                        ex, c0, ncn = items[n]
                        wg_, kwg, wu_, kwu, wd_, kwd = loaded[n]
                        for q in range(4):
                            t = 4 * g + q
                            for hf in range(2):
                                bk, bt = nb()
                                for cc in range(ncn):
                                    mm(bk[:], actb[:, cc, t * 128:(t + 1) * 128], wd_[:, cc, hf * 512:(hf + 1) * 512],
                                       [kact[g], kwd], [bt], start=(cc == 0), stop=(cc == ncn - 1))
                                xs = X[:, t, hf * 512:(hf + 1) * 512]
                                if moe:
                                    stt(xs, bk[:], comb[:, t, ex:ex + 1], xs, ALU.mult, ALU.add, [bt, kcomb[t], tX[t]], [tX[t]])
                                else:
                                    tt(xs, bk[:], xs, ALU.add, [bt, tX[t]], [tX[t]])

                    pend = None
                    for n in range(len(items)):
                        for g in range(NG):
                            up_phase(n, g)
                            if pend is not None:
                                down_phase(*pend)
                                if pend[1] == NG - 1:
                                    load_w(pend[0] + 2)
                            pend = (n, g)
                    down_phase(*pend)
                    last = (l == n_layers - 1)
                    outv = out.rearrange("(t p) d -> p t d", p=128)
                    def store_tile(t):
                        S.dma("sync", outv[:, t, :], X[:, t, :], dout, reads=[tX[t]])
                    def ln2_group(g):
                        run_il([ln_gen(4 * g + q, g2, b2_, kLP, sm, after=(store_tile if last else None))
                                for q in range(4)], 4)
                    for g in range(NG):
                        ln2_group(g)
                        if not last:
                            make_xt(g)
                    S.barrier(rotate=not last)
            elif dbg:
                pass

        if "F" not in steps:
            outv = out.rearrange("(t p) d -> p t d", p=128)
            for t in range(NT):
                S.dma("sync", outv[:, t, :], X[:, t, :], dout, reads=[tX[t]])
        S.wait_events("sync", [(dout.name, dout.count)])
        S.barrier()
        print("instr counts", S.ninst, "gen", S.gen)
    return nc


def prep_inputs(inputs, n_layers=DEPTH):
    f = lambda a: np.ascontiguousarray(np.asarray(a, dtype=np.float32))
    shared = {}
    for k in ("w_in", "a_ln_g", "a_ln_b", "b_a_log", "b_dt_bias", "b_norm_g", "w_out", "ln1_g", "ln1_b", "ln2_g",
              "ln2_b", "ffn_w_gate", "ffn_w_up", "ffn_w_down", "moe_router", "moe_w_gate", "moe_w_up", "moe_w_down"):
        shared[k] = f(inputs[k])
    cw = np.asarray(inputs["conv_w"], np.float32)
    shared["conv_w"] = f(cw.reshape(DEPTH, 4, 12, 128).transpose(0, 3, 2, 1))
    ws = np.asarray(inputs["a_ws"], np.float32)
    shared["a_ws"] = f(ws.transpose(0, 3, 1, 2))
    shared["a_bs"] = f(np.asarray(inputs["a_bs"], np.float32).transpose(0, 2, 1))
    if n_layers < DEPTH:
        for k in list(shared.keys()):
            shp = small_shape(k, IN_SHAPES[k], n_layers)
            sl = tuple(slice(0, n) for n in shp)
            shared[k] = f(shared[k][sl])
    shared.update(host_consts())
    x = np.asarray(inputs["x"], np.float32)
    in_maps = []
    for b in range(8):
        m = dict(shared)
        m["x"] = f(x[b])
        in_maps.append(m)
    return in_maps


def kernel(**inputs):
    in_maps = prep_inputs(inputs)
    nc = build()
    res = run_bass_kernel_spmd(nc, in_maps, core_ids=list(range(8)))
    return np.stack([np.asarray(r["out"], dtype=np.float32) for r in res.results], axis=0)
```
